# Optimizing a Trainium2 kernel written in Bass

```python
import jax
import jax.numpy as jnp
from jax import lax
import numpy as np

D_MODEL = 1024
BATCH = 16
SEQ = 2048
DEPTH = 4

GRID_W = 64
CTX_LEN = 256
NORM_EPS = 1e-6

ATT_HEADS = 8
ATT_KV_HEADS = 2
ATT_HEAD_DIM = 64
ATT_GROUP = ATT_HEADS // ATT_KV_HEADS
ATT_WIDTH = ATT_HEADS * ATT_HEAD_DIM
ATT_KV_WIDTH = ATT_KV_HEADS * ATT_HEAD_DIM
ROPE_THETA = 10000.0
Q_BLOCK = 128

RWKV_HEADS = 8
RWKV_HEAD_DIM = 64
RWKV_WIDTH = RWKV_HEADS * RWKV_HEAD_DIM
DECAY_RANK = 64
ICLR_RANK = 64
GATE_RANK = 128
RWKV_GN_EPS = 64e-5

CONV_WIDTH = 512
CONV_K = 3

N_BRANCHES = 3

N_EXPERTS = 16
EXPERT_FF = 1024
CAPACITY_FACTOR = 2

ATT_SPLITS = [ATT_WIDTH, ATT_KV_WIDTH, ATT_KV_WIDTH]
RWKV_SPLITS = [RWKV_WIDTH, RWKV_WIDTH, RWKV_WIDTH, 2 * DECAY_RANK, 2 * ICLR_RANK, GATE_RANK]
CONV_SPLITS = [CONV_WIDTH, CONV_WIDTH, CONV_WIDTH]
GROUP_SPLITS = [sum(ATT_SPLITS), sum(RWKV_SPLITS), sum(CONV_SPLITS), N_BRANCHES * D_MODEL]
RWKV_SEG = sum(RWKV_SPLITS)
IN_WIDTH = sum(GROUP_SPLITS)

kernel_name = 'hybrid_flow_rwkv_gqa_conv_ecmoe'


def _split(x, sizes):
    return jnp.split(x, np.cumsum(sizes)[:-1].tolist(), axis=-1)


def _rms_norm(x, gain):
    xf = x.astype(jnp.float32)
    y = xf * lax.rsqrt(jnp.mean(xf * xf, axis=-1, keepdims=True) + NORM_EPS)
    return (y * gain.astype(jnp.float32)).astype(x.dtype)


def _modulate(x, gain, shift, scale):
    return _rms_norm(x, gain) * (1 + scale) + shift


def _neighbours(u):
    up = jnp.pad(u, ((0, 0), (1, 1), (0, 0)))
    return up[:, :-2], up[:, 2:]


def _axial_rope_tables(n_tokens, dtype):
    rows = n_tokens // GRID_W
    row = jnp.repeat(jnp.arange(rows, dtype=jnp.float32), GRID_W)
    col = jnp.tile(jnp.arange(GRID_W, dtype=jnp.float32), rows)
    axis_dim = ATT_HEAD_DIM // 2
    inv_freq = ROPE_THETA ** (-jnp.arange(0, axis_dim, 2, dtype=jnp.float32) / axis_dim)
    ang_r = row[:, None] * inv_freq[None, :]
    ang_c = col[:, None] * inv_freq[None, :]
    return (jnp.cos(ang_r).astype(dtype), jnp.sin(ang_r).astype(dtype),
            jnp.cos(ang_c).astype(dtype), jnp.sin(ang_c).astype(dtype))


def _rotate_half(x, cos, sin):
    x1, x2 = jnp.split(x, 2, axis=-1)
    cos = cos[:, None, :]
    sin = sin[:, None, :]
    return jnp.concatenate([x1 * cos - x2 * sin, x1 * sin + x2 * cos], axis=-1)


def _axial_rotary(x, tables):
    cos_r, sin_r, cos_c, sin_c = tables
    x_row, x_col = jnp.split(x, 2, axis=-1)
    return jnp.concatenate([_rotate_half(x_row, cos_r, sin_r), _rotate_half(x_col, cos_c, sin_c)], axis=-1)


def _attend(q, k, v):
    s = jnp.einsum('bqkgd,bskd->bkgqs', q, k).astype(jnp.float32) * (ATT_HEAD_DIM ** -0.5)
    p = jax.nn.softmax(s, axis=-1).astype(v.dtype)
    return jnp.einsum('bkgqs,bskd->bqkgd', p, v)


def _latent_attention(q, k_all, v_all):
    b, t = q.shape[:2]
    n_blocks = t // Q_BLOCK
    q_blocks = jnp.moveaxis(q.reshape(b, n_blocks, Q_BLOCK, ATT_KV_HEADS, ATT_GROUP, ATT_HEAD_DIM), 1, 0)
    o = lax.map(lambda q_blk: _attend(q_blk, k_all, v_all), q_blocks)
    return jnp.moveaxis(o, 0, 1).reshape(b, t, ATT_WIDTH)


def _attention_branch(att_lat, att_ctx, q_gain, k_gain, need_ctx):
    def heads(seg):
        b, t = seg.shape[:2]
        q, k, v = _split(seg, ATT_SPLITS)
        q = _rms_norm(q.reshape(b, t, ATT_HEADS, ATT_HEAD_DIM), q_gain)
        k = _rms_norm(k.reshape(b, t, ATT_KV_HEADS, ATT_HEAD_DIM), k_gain)
        return q, k, v.reshape(b, t, ATT_KV_HEADS, ATT_HEAD_DIM)

    b, t = att_lat.shape[:2]
    q_l, k_l, v_l = heads(att_lat)
    q_c, k_c, v_c = heads(att_ctx)
    tables = _axial_rope_tables(t, q_l.dtype)
    q_l = _axial_rotary(q_l, tables).reshape(b, t, ATT_KV_HEADS, ATT_GROUP, ATT_HEAD_DIM)
    k_l = _axial_rotary(k_l, tables)
    k_all = jnp.concatenate([k_c, k_l], axis=1)
    v_all = jnp.concatenate([v_c, v_l], axis=1)
    o_lat = _latent_attention(q_l, k_all, v_all)
    if not need_ctx:
        return o_lat, None
    n_ctx = att_ctx.shape[1]
    q_c = q_c.reshape(b, n_ctx, ATT_KV_HEADS, ATT_GROUP, ATT_HEAD_DIM)
    o_ctx = _attend(q_c, k_c, v_c).reshape(b, n_ctx, ATT_WIDTH)
    return o_lat, o_ctx


def _to_heads(u):
    return u.reshape(u.shape[:-1] + (RWKV_HEADS, RWKV_HEAD_DIM))


def _l2_heads(u):
    uf = u.astype(jnp.float32)
    return (uf * lax.rsqrt(jnp.sum(uf * uf, axis=-1, keepdims=True) + 1e-12)).astype(u.dtype)


def _rwkv_prepare(seg, mu, decay_w0, decay_w2, iclr_a0, iclr_a2, gate_g2, k_k, k_a):
    b, t, _ = seg.shape
    prev, nxt = _neighbours(seg)
    seg = seg + mu * (0.5 * (prev + nxt) - seg)
    r, k, v, w_lo, a_lo, g_lo = _split(seg, RWKV_SPLITS)
    w_pre = decay_w0 + jnp.einsum('btdr,drc->btdc', jnp.tanh(w_lo.reshape(b, t, 2, DECAY_RANK)), decay_w2)
    decay = jnp.exp(-jnp.exp(-jax.nn.softplus(-w_pre) - 0.5))
    a = jax.nn.sigmoid(iclr_a0 + jnp.einsum('btdr,drc->btdc', a_lo.reshape(b, t, 2, ICLR_RANK), iclr_a2))
    g = jax.nn.sigmoid(g_lo) @ gate_g2
    kk = _l2_heads(_to_heads(k * k_k))
    k_dir = k[:, :, None, :] * (1 + (a - 1) * k_a)
    return (_to_heads(r), _to_heads(k), _to_heads(v), g, _to_heads(decay), _to_heads(k_dir), kk, _to_heads(a))


def _time_major(x_fwd, x_bwd):
    return jnp.moveaxis(jnp.stack([x_fwd, jnp.flip(x_bwd, axis=1)], axis=0), 2, 0)


def _rwkv_bidir_scan(r, v, decay, k_dir, kk, a, s0):
    b_vec = kk[:, :, None] * a
    xs = (_time_major(r, r), _time_major(decay[:, :, 0], decay[:, :, 1]),
          _time_major(k_dir[:, :, 0], k_dir[:, :, 1]), _time_major(v, v),
          _time_major(-kk, -kk), _time_major(b_vec[:, :, 0], b_vec[:, :, 1]))

    def step(state, inp):
        r_t, w_t, k_t, v_t, a_t, b_t = inp
        sa = jnp.einsum('dbhvk,dbhk->dbhv', state, a_t)
        state = state * w_t[..., None, :] + sa[..., :, None] * b_t[..., None, :] + v_t[..., :, None] * k_t[..., None, :]
        return state, jnp.einsum('dbhvk,dbhk->dbhv', state, r_t)

    s_final, ys = lax.scan(step, s0, xs)
    y = ys[:, 0] + jnp.flip(ys[:, 1], axis=0)
    return jnp.moveaxis(y, 0, 1), s_final


def _rwkv_readout(y, r, k, v, g, r_k, gn_w, gn_b):
    b, t = y.shape[:2]
    yf = y.astype(jnp.float32)
    mean = jnp.mean(yf, axis=-1, keepdims=True)
    var = jnp.mean(jnp.square(yf - mean), axis=-1, keepdims=True)
    yn = ((yf - mean) * lax.rsqrt(var + RWKV_GN_EPS)).astype(y.dtype).reshape(b, t, RWKV_WIDTH)
    bonus = jnp.sum(r * k * _to_heads(r_k), axis=-1, keepdims=True) * v
    return (yn * gn_w + gn_b + bonus.reshape(b, t, RWKV_WIDTH)) * g


def _rwkv_branch(seg_lat, seg_ctx, need_ctx, mu, decay_w0, decay_w2, iclr_a0, iclr_a2, gate_g2, k_k, k_a, r_k, gn_w, gn_b):
    r_c, k_c, v_c, g_c, w_c, kd_c, kk_c, a_c = _rwkv_prepare(seg_ctx, mu, decay_w0, decay_w2, iclr_a0, iclr_a2, gate_g2, k_k, k_a)
    s0 = jnp.zeros((2, seg_ctx.shape[0], RWKV_HEADS, RWKV_HEAD_DIM, RWKV_HEAD_DIM), r_c.dtype)
    y_c, s_ctx = _rwkv_bidir_scan(r_c, v_c, w_c, kd_c, kk_c, a_c, s0)
    r_l, k_l, v_l, g_l, w_l, kd_l, kk_l, a_l = _rwkv_prepare(seg_lat, mu, decay_w0, decay_w2, iclr_a0, iclr_a2, gate_g2, k_k, k_a)
    y_l, _ = _rwkv_bidir_scan(r_l, v_l, w_l, kd_l, kk_l, a_l, s_ctx)
    o_lat = _rwkv_readout(y_l, r_l, k_l, v_l, g_l, r_k, gn_w, gn_b)
    if not need_ctx:
        return o_lat, None
    return o_lat, _rwkv_readout(y_c, r_c, k_c, v_c, g_c, r_k, gn_w, gn_b)


def _short_conv(seg, conv_w):
    b_gate, c_gate, u = _split(seg, CONV_SPLITS)
    z = c_gate * u
    z_prev, z_next = _neighbours(z)
    return b_gate * (conv_w[0] * z_prev + conv_w[1] * z + conv_w[2] * z_next)


def _mixer(h_lat, h_ctx, need_ctx, w_in, q_gain, k_gain, shift_mu, decay_w0, decay_w2, iclr_a0, iclr_a2,
           gate_g2, rwkv_kk, rwkv_ka, rwkv_rk, rwkv_gn_w, rwkv_gn_b, conv_w, w_br_att, w_br_rwkv, w_br_conv, w_out):
    att_l, rw_l, cv_l, gt_l = _split(h_lat @ w_in, GROUP_SPLITS)
    att_c, rw_c, cv_c, gt_c = _split(h_ctx @ w_in, GROUP_SPLITS)
    o_att_l, o_att_c = _attention_branch(att_l, att_c, q_gain, k_gain, need_ctx)
    o_rw_l, o_rw_c = _rwkv_branch(rw_l, rw_c, need_ctx, shift_mu, decay_w0, decay_w2, iclr_a0, iclr_a2,
                                  gate_g2, rwkv_kk, rwkv_ka, rwkv_rk, rwkv_gn_w, rwkv_gn_b)

    def merge(gates, o_att, o_rwkv, o_conv):
        g_att, g_rwkv, g_conv = jnp.split(jax.nn.sigmoid(gates), N_BRANCHES, axis=-1)
        m = g_att * (o_att @ w_br_att) + g_rwkv * (o_rwkv @ w_br_rwkv) + g_conv * (o_conv @ w_br_conv)
        return m @ w_out

    y_lat = merge(gt_l, o_att_l, o_rw_l, _short_conv(cv_l, conv_w))
    if not need_ctx:
        return y_lat, None
    return y_lat, merge(gt_c, o_att_c, o_rw_c, _short_conv(cv_c, conv_w))


def _expert_choice_ffn(h, w_router, w_gate, w_up, w_down):
    b, n, d = h.shape
    cap = CAPACITY_FACTOR * n // N_EXPERTS
    aff = jax.nn.softmax(jnp.einsum('bnd,de->bne', h, w_router).astype(jnp.float32), axis=-1)
    gate, idx = lax.top_k(jnp.swapaxes(aff, 1, 2), cap)
    batch_idx = jnp.arange(b)[:, None, None]
    xe = h[batch_idx, idx]
    hid = jax.nn.silu(jnp.einsum('becd,edf->becf', xe, w_gate)) * jnp.einsum('becd,edf->becf', xe, w_up)
    ye = jnp.einsum('becf,efd->becd', hid, w_down) * gate[..., None].astype(h.dtype)
    flat = (batch_idx * n + idx).reshape(-1)
    out = jnp.zeros((b * n, d), h.dtype).at[flat].add(ye.reshape(-1, d))
    return out.reshape(b, n, d)


def setup_inputs(seed: int = 0) -> dict:
    key = jax.random.key(seed)
    ks = jax.random.split(key, 40)
    f32 = jnp.float32
    L = DEPTH
    D = D_MODEL

    def nrm(k, shape, scale):
        return jax.random.normal(k, shape, f32) * scale

    def gain(k, shape, base=1.0):
        return base + 0.05 * jax.random.normal(k, shape, f32)

    return {
        'x': nrm(ks[0], (BATCH, SEQ, D), 1.0),
        'c': nrm(ks[1], (BATCH, D), 1.0),
        'ctx': nrm(ks[2], (BATCH, CTX_LEN, D), 1.0),
        'c_ctx': nrm(ks[3], (D,), 1.0),
        'ada_w': nrm(ks[4], (L, D, 6 * D), 0.5 * D ** -0.5),
        'ada_b': nrm(ks[5], (L, 6 * D), 0.02),
        'norm1': gain(ks[6], (L, D)),
        'w_in': nrm(ks[7], (L, D, IN_WIDTH), D ** -0.5),
        'q_gain': gain(ks[8], (L, ATT_HEAD_DIM)),
        'k_gain': gain(ks[9], (L, ATT_HEAD_DIM)),
        'shift_mu': jax.random.uniform(ks[10], (L, RWKV_SEG), f32),
        'decay_w0': jax.random.uniform(ks[11], (L, 2, RWKV_WIDTH), f32, -4.0, 1.0),
        'decay_w2': nrm(ks[12], (L, 2, DECAY_RANK, RWKV_WIDTH), 0.1 * DECAY_RANK ** -0.5),
        'iclr_a0': nrm(ks[13], (L, 2, RWKV_WIDTH), 0.5),
        'iclr_a2': nrm(ks[14], (L, 2, ICLR_RANK, RWKV_WIDTH), 0.5 * ICLR_RANK ** -0.5),
        'gate_g2': nrm(ks[15], (L, GATE_RANK, RWKV_WIDTH), GATE_RANK ** -0.5),
        'rwkv_kk': gain(ks[16], (L, RWKV_WIDTH), 0.85),
        'rwkv_ka': gain(ks[17], (L, RWKV_WIDTH)),
        'rwkv_rk': nrm(ks[18], (L, RWKV_WIDTH), 0.1),
        'rwkv_gn_w': gain(ks[19], (L, RWKV_WIDTH)),
        'rwkv_gn_b': nrm(ks[20], (L, RWKV_WIDTH), 0.02),
        'conv_w': nrm(ks[21], (L, CONV_K, CONV_WIDTH), CONV_K ** -0.5),
        'w_br_att': nrm(ks[22], (L, ATT_WIDTH, D), ATT_WIDTH ** -0.5),
        'w_br_rwkv': nrm(ks[23], (L, RWKV_WIDTH, D), RWKV_WIDTH ** -0.5),
        'w_br_conv': nrm(ks[24], (L, CONV_WIDTH, D), CONV_WIDTH ** -0.5),
        'w_out': nrm(ks[25], (L, D, D), D ** -0.5),
        'norm2': gain(ks[26], (L, D)),
        'w_router': nrm(ks[27], (L, D, N_EXPERTS), D ** -0.5),
        'exp_gate': nrm(ks[28], (L, N_EXPERTS, D, EXPERT_FF), D ** -0.5),
        'exp_up': nrm(ks[29], (L, N_EXPERTS, D, EXPERT_FF), D ** -0.5),
        'exp_down': nrm(ks[30], (L, N_EXPERTS, EXPERT_FF, D), EXPERT_FF ** -0.5),
        'final_norm': gain(ks[31], (D,)),
    }


def reference(x, c, ctx, c_ctx, ada_w, ada_b, norm1, w_in, q_gain, k_gain, shift_mu, decay_w0, decay_w2,
              iclr_a0, iclr_a2, gate_g2, rwkv_kk, rwkv_ka, rwkv_rk, rwkv_gn_w, rwkv_gn_b, conv_w,
              w_br_att, w_br_rwkv, w_br_conv, w_out, norm2, w_router, exp_gate, exp_up, exp_down, final_norm):
    silu_c = jax.nn.silu(c)
    silu_cc = jax.nn.silu(c_ctx)
    for layer in range(DEPTH):
        need_ctx = layer < DEPTH - 1
        mod_l = (silu_c @ ada_w[layer] + ada_b[layer])[:, None, :]
        mod_c = silu_cc @ ada_w[layer] + ada_b[layer]
        sh1, sc1, g1, sh2, sc2, g2 = jnp.split(mod_l, 6, axis=-1)
        sh1c, sc1c, g1c, sh2c, sc2c, g2c = jnp.split(mod_c, 6, axis=-1)

        h_l = _modulate(x, norm1[layer], sh1, sc1)
        h_c = _modulate(ctx, norm1[layer], sh1c, sc1c)
        y_l, y_c = _mixer(h_l, h_c, need_ctx, w_in[layer], q_gain[layer], k_gain[layer], shift_mu[layer],
                          decay_w0[layer], decay_w2[layer], iclr_a0[layer], iclr_a2[layer], gate_g2[layer],
                          rwkv_kk[layer], rwkv_ka[layer], rwkv_rk[layer], rwkv_gn_w[layer], rwkv_gn_b[layer],
                          conv_w[layer], w_br_att[layer], w_br_rwkv[layer], w_br_conv[layer], w_out[layer])
        x = x + g1 * y_l
        h_l = _modulate(x, norm2[layer], sh2, sc2)
        x = x + g2 * _expert_choice_ffn(h_l, w_router[layer], exp_gate[layer], exp_up[layer], exp_down[layer])

        if need_ctx:
            ctx = ctx + g1c * y_c
            h_c = _modulate(ctx, norm2[layer], sh2c, sc2c)
            ctx = ctx + g2c * _expert_choice_ffn(h_c, w_router[layer], exp_gate[layer], exp_up[layer], exp_down[layer])
    return _rms_norm(x, final_norm)
```

```python
import contextlib
import os
import numpy as np
import concourse.bass as bass
import concourse.mybir as mybir
from concourse.bass_utils import run_bass_kernel_spmd

F32 = mybir.dt.float32
BF16 = mybir.dt.bfloat16
ALU = mybir.AluOpType
AF = mybir.ActivationFunctionType
AX = mybir.AxisListType

D = 1024
KD = 8
INW = 7296
O_ATT, O_RW, O_CV, O_GT = 0, 768, 2688, 4224
NE = 16
DECAY_C = -0.6065306597126334
EMBED_WAIT = True


class Buf:
    __slots__ = ("name", "w", "r")

    def __init__(self, name=""):
        self.name = name
        self.w = None
        self.r = {}


class Tile:
    __slots__ = ("ap", "b")

    def __init__(self, ap, b):
        self.ap = ap
        self.b = b


class _Rec:
    def __init__(self):
        self.call = None

    def __getattr__(self, name):
        def f(*a, **k):
            self.call = (name, a, k)
            return self
        return f


class Em:
    ENG = ("pe", "act", "dve", "pool", "sp")
    NDMA = 8
    RESET_AT = 1 << 40

    def __init__(self, nc, stack):
        self.nc = nc
        self.sems = []
        self.val = []
        for e in self.ENG:
            self.sems.append(stack.enter_context(nc.semaphore("s_" + e)))
            self.val.append(0)
        self.cidx = {e: i for i, e in enumerate(self.ENG)}
        self.dma_pool = {}
        self.dma_rr = {}
        for q in ("sp", "pool"):
            ids = []
            for j in range(self.NDMA):
                self.sems.append(stack.enter_context(nc.semaphore(f"d_{q}{j}")))
                self.val.append(0)
                ids.append(len(self.sems) - 1)
            self.dma_pool[q] = ids
            self.dma_rr[q] = 0
        self.B1 = stack.enter_context(nc.semaphore("bar1"))
        self.B2 = stack.enter_context(nc.semaphore("bar2"))
        self.nreset = 0
        self.epoch = 0
        self.known = {e: {} for e in self.ENG}
        self.prog = {e: [] for e in self.ENG}
        self.ninst = 0
        self.muted = False

    def _waits(self, e, reads, writes, extra=()):
        need = {}

        def add(s, v):
            if need.get(s, 0) < v:
                need[s] = v
        for s, v in extra:
            add(s, v)
        ep = self.epoch
        for b in reads:
            if b.w is not None and b.w[2] == ep:
                add(b.w[0], b.w[1])
        for b in writes:
            if b.w is not None and b.w[2] == ep:
                add(b.w[0], b.w[1])
            for s, (v, e_) in b.r.items():
                if e_ == ep:
                    add(s, v)
        kn = self.known[e]
        own = self.cidx[e]
        out = []
        for s, v in need.items():
            if kn.get(s, 0) >= v:
                continue
            kn[s] = v
            if e == "pe" and s == own:
                continue
            out.append((s, v))
        return out

    def _mark(self, ev, reads, writes):
        s, v = ev
        ep = self.epoch
        for b in reads:
            cur = b.r.get(s)
            if cur is None or cur[1] != ep or cur[0] < v:
                b.r[s] = (v, ep)
        for b in writes:
            b.w = (s, v, ep)
            b.r = {}

    def op(self, e, fn, reads=(), writes=()):
        if self.muted:
            return None
        if max(self.val) >= self.RESET_AT:
            self.reset()
        w = self._waits(e, reads, writes)
        s = self.cidx[e]
        self.val[s] += 1
        ev = (s, self.val[s])
        rec = _Rec()
        fn(rec)
        name_, a_, k_ = rec.call
        self.prog[e].append((w, (lambda g, name_=name_, a_=a_, k_=k_: getattr(g, name_)(*a_, **k_)), s, 1))
        self._mark(ev, reads, writes)
        self.ninst += 1
        return ev

    def dma(self, q, out, in_, reads=(), writes=(), **kw):
        if self.muted:
            return None
        if max(self.val) >= self.RESET_AT:
            self.reset()
        ids = self.dma_pool[q]
        s = ids[self.dma_rr[q] % len(ids)]
        self.dma_rr[q] += 1
        w = self._waits(q, reads, writes, extra=[(s, self.val[s])] if self.val[s] else [])
        self.val[s] += 16
        ev = (s, self.val[s])
        self.prog[q].append((w, (lambda e, out=out, in_=in_, kw=kw: e.dma_start(out=out, in_=in_, **kw)), s, 16))
        self._mark(ev, reads, writes)
        self.ninst += 1
        return ev

    def barrier(self):
        for e in self.ENG:
            kn = self.known[e]
            w = []
            for s in range(len(self.sems)):
                v = self.val[s]
                if v > 0 and kn.get(s, 0) < v:
                    w.append((s, v))
                    kn[s] = v
            if w:
                self.prog[e].append((w, None, None, 0))

    def reset(self):
        self.barrier()
        self.nreset += 1
        k = self.nreset
        B1, B2, sems = self.B1, self.B2, self.sems
        for e in self.ENG:
            self.prog[e].append(([], (lambda g: g.nop().then_inc(B1, 1)), None, 0))
        self.prog["pool"].append(([(B1, 5 * k)], (lambda g: [g.sem_clear(h) for h in sems]), None, 0))
        self.prog["pool"].append(([], (lambda g: g.nop().then_inc(B2, 1)), None, 0))
        for e in self.ENG:
            self.prog[e].append(([(B2, k)], None, None, 0))
        self.val = [0] * len(self.val)
        self.known = {e: {} for e in self.ENG}
        self.epoch += 1

    def finalize(self):
        nc = self.nc
        sems = self.sems
        prog = self.prog

        def run(eng, lst):
            for (w, fn, s, inc) in lst:
                emb = None
                if fn is not None and s is not None and w and EMBED_WAIT:
                    emb, w = w[0], w[1:]
                for (ws, wv) in w:
                    eng.wait_ge(sems[ws] if isinstance(ws, int) else ws, wv)
                if fn is not None:
                    ins = fn(eng)
                    if emb is not None:
                        ins._wait_ge(sems[emb[0]], emb[1])
                    if s is not None:
                        ins.then_inc(sems[s], inc)
        with nc.Block() as block:
            @block.tensor
            def _(e):
                run(e, prog["pe"])

            @block.scalar
            def _(e):
                run(e, prog["act"])

            @block.vector
            def _(e):
                run(e, prog["dve"])

            @block.gpsimd
            def _(e):
                run(e, prog["pool"])

            @block.sync
            def _(e):
                run(e, prog["sp"])


class Arena:
    def __init__(self, nc, st, nbytes):
        self.t = st.enter_context(nc.sbuf_tensor("arena", [128, nbytes // 4], F32))
        self.cap = nbytes
        self.off = 0
        self.base = 0

    def alloc(self, shape, dt=F32, name=""):
        esz = 4 if dt == F32 else 2
        nel = int(np.prod(shape[1:]))
        n = (nel * esz + 63) // 64 * 64
        off = self.off
        self.off += n
        assert self.off <= self.cap, f"SBUF arena overflow {self.off} > {self.cap} ({name})"
        ap = self.t[0:shape[0], off // 4:(off + n) // 4]
        if dt != F32:
            ap = ap.bitcast(dt)
        ap = ap[:, 0:nel]
        if len(shape) > 2:
            names = [f"a{i}" for i in range(len(shape) - 1)]
            pat = "p (" + " ".join(names) + ") -> p " + " ".join(names)
            ap = ap.rearrange(pat, **{nm: int(s) for nm, s in zip(names[:-1], shape[1:-1])})
        return Tile(ap, Buf(name))

    def mark_base(self):
        self.base = self.off

    def reset(self):
        self.off = self.base


def dap(t, offset, dims):
    return bass.AP(t.tensor, int(offset), [[int(s), int(c)] for s, c in dims])


class _Stop(Exception):
    pass


def build(T=2048, C=256, L=4, NB=2, dump=(), upto=99):
    NT = T + C
    NTL, NTC, NTT = T // 128, C // 128, (T + C) // 128
    ZR = NT + 3
    capL, capC = 2 * T // NE, 2 * C // NE
    NS = capL + capC
    JL = (capL + 127) // 128
    JW = min(128, capL)
    QB = min(512, T)

    nc = bass.Bass("TRN2", target_bir_lowering=False)

    def din(name, shape):
        return nc.dram_tensor(name, list(shape), F32, kind="ExternalInput").ap()

    I = {}
    I["x"] = din("x", [NB, T, D])
    I["ctx"] = din("ctx", [NB, C, D])
    I["c"] = din("c", [NB, D])
    I["c_ctx"] = din("c_ctx", [1, D])
    for nm, sh in [("ada_w", [L, D, 6 * D]), ("ada_b", [L, 6 * D]), ("norm1", [L, D]), ("w_in", [L, D, INW]),
                   ("q_gain", [L, 64]), ("k_gain", [L, 64]), ("shift_mu", [L, 1920]), ("decay_w0", [L, 2, 512]),
                   ("decay_w2", [L, 2, 64, 512]), ("iclr_a0", [L, 2, 512]), ("iclr_a2", [L, 2, 64, 512]),
                   ("gate_g2", [L, 128, 512]), ("rwkv_kk", [L, 512]), ("rwkv_ka", [L, 512]), ("rwkv_rk", [L, 512]),
                   ("rwkv_gn_w", [L, 512]), ("rwkv_gn_b", [L, 512]), ("conv_w", [L, 3, 512]),
                   ("w_br_att", [L, 512, D]), ("w_br_rwkv", [L, 512, D]), ("w_br_conv", [L, 512, D]),
                   ("w_out", [L, D, D]), ("norm2", [L, D]), ("w_router", [L, D, NE]),
                   ("exp_gate", [L, NE, D, D]), ("exp_up", [L, NE, D, D]), ("exp_down", [L, NE, D, D]),
                   ("final_norm", [1, D]), ("rope", [T, 64]), ("cst", [128, 768])]:
        I[nm] = din(nm, sh)
    OUT = nc.dram_tensor("out", [NB, T, D], F32, kind="ExternalOutput").ap()

    def dscr(name, shape, dt=F32):
        if name in dump:
            return nc.dram_tensor(name, list(shape), dt, kind="ExternalOutput").ap()
        return nc.dram_tensor(name, list(shape), dt).ap()

    XS = dscr("XS", [NB, NT, D])
    ZIN = dscr("ZIN", [NB, ZR, INW])
    MODV = dscr("MODV", [L, 3, 6 * D])
    SI = dscr("SI", [NB * 16, NT, 6, 64])
    YO = dscr("YO", [4, NB * 16, NT, 16])
    RWAUX = dscr("RWAUX", [NB, NT, 1024])
    OBR = dscr("OBR", [NB, NT, 3, 512])
    YE = dscr("YE", [NE, NS, D], BF16)

    def zrow(n):
        return n + 1 if n < C else n + 2

    with contextlib.ExitStack() as st:
        em = Em(nc, st)
        stage_ctr = [0]

        dbg_done = set()

        def dbg(name, tile_, shape, dt=F32):
            if 'DBG' not in dump or name in dbg_done:
                return
            dbg_done.add(name)
            o = nc.dram_tensor('dbg_' + name, list(shape), dt, kind='ExternalOutput').ap()
            em.dma('sp', o, tile_.ap, reads=[tile_.b])

        def stop(label):
            if os.environ.get('K_STOP') == label:
                em.muted = True

        def chk():
            stage_ctr[0] += 1
            if stage_ctr[0] > upto:
                em.muted = True
        AR = Arena(nc, st, 184 * 1024)
        PS = []
        for i in range(8):
            t = st.enter_context(nc.psum_tensor(f"ps{i}", [128, 512], F32))
            PS.append(Tile(t[:, :], Buf(f"ps{i}")))
        ps_rr = [0]

        def psum():
            p = PS[4 + ps_rr[0] % 4]
            ps_rr[0] += 1
            return p

        def bfv(p):
            return p.ap.bitcast(BF16)

        def V(e, fn, r=(), w=()):
            return em.op(e, fn, reads=[t.b for t in r], writes=[t.b for t in w])

        def LD(out_t, out_ap, in_ap, q="sp"):
            return em.dma(q, out_ap, in_ap, writes=[out_t.b])

        def STO(in_t, out_ap, in_ap, q="pool"):
            return em.dma(q, out_ap, in_ap, reads=[in_t.b])

        def tt(e, out_t, out, in0_t, in0, in1_t, in1, op):
            return V(e, lambda g: g.tensor_tensor(out=out, in0=in0, in1=in1, op=op), r=[in0_t, in1_t], w=[out_t])

        def bc_row(dram_ap_1d_offset, tensor_ap, n, parts=128):
            return dap(tensor_ap, dram_ap_1d_offset, [[0, parts], [1, n]])

        cst = AR.alloc([128, 768], F32, "cst")
        LD(cst, cst.ap, I["cst"])
        ident = cst.ap[:, 0:128]
        Jm = cst.ap[:, 128:256]
        iota = cst.ap[:, 512:768]
        cstb = AR.alloc([128, 512], BF16, "cstb")
        V("dve", lambda g: g.tensor_copy(out=cstb.ap, in_=cst.ap[:, 0:512]), r=[cst], w=[cstb])
        ident_b = cstb.ap[:, 0:128]
        U_b = cstb.ap[:, 256:384]
        ones_b = cstb.ap[:, 384:512]
        rope = AR.alloc([128, NTL, 64], F32, "rope")
        LD(rope, rope.ap, I["rope"].rearrange("(i p) f -> p i f", p=128))
        AR.mark_base()

        for b in range(NB):
            em.dma("sp", XS[b, 0:C, :], I["ctx"][b])
            em.dma("sp", XS[b, C:NT, :], I["x"][b])
        zt = AR.alloc([1, INW], F32, "zt")
        V("dve", lambda g: g.memset(zt.ap, 0.0), w=[zt])
        for b in range(NB):
            for r in (0, C + 1, ZR - 1):
                STO(zt, ZIN[b, r:r + 1, :], zt.ap)
        c2 = AR.alloc([3, D], F32, "c2")
        for b in range(NB):
            LD(c2, c2.ap[b:b + 1, :], I["c"][b:b + 1, :])
        LD(c2, c2.ap[2:3, :], I["c_ctx"])
        V("act", lambda g: g.activation(out=c2.ap, in_=c2.ap, func=AF.Silu), r=[c2], w=[c2])
        cT = AR.alloc([128, 8, 3], BF16, "cT")
        p = psum()
        for k in range(8):
            V("pe", lambda g, k=k: g.transpose(out=p.ap[:, k * 3:(k + 1) * 3], in_=c2.ap[0:3, k * 128:(k + 1) * 128],
                                               identity=ident[0:3, 0:3]), r=[c2, cst], w=[p])
        V("dve", lambda g: g.tensor_copy(out=cT.ap, in_=p.ap[:, 0:24].rearrange("p (k v) -> p k v", v=3)), r=[p], w=[cT])
        wb = [AR.alloc([128, 8, 512], BF16, f"adaw{i}") for i in range(2)]
        bb = [AR.alloc([3, 512], F32, f"adab{i}") for i in range(2)]
        mo = [AR.alloc([3, 512], F32, f"mo{i}") for i in range(2)]
        it = 0
        for l in range(L):
            for j in range(12):
                w_, b_, m_ = wb[it % 2], bb[it % 2], mo[it % 2]
                it += 1
                LD(w_, w_.ap, I["ada_w"][l, :, j * 512:(j + 1) * 512].rearrange("(k p) n -> p k n", p=128), q="pool")
                LD(b_, b_.ap, bc_row(l * 6 * D + j * 512, I["ada_b"], 512, parts=3))
                p = psum()
                for k in range(8):
                    V("pe", lambda g, k=k, p=p, w_=w_: g.matmul(p.ap[0:3, :], lhsT=cT.ap[:, k, :], rhs=w_.ap[:, k, :],
                                                             start=(k == 0), stop=(k == 7)), r=[cT, w_], w=[p])
                tt("dve", m_, m_.ap, p, p.ap[0:3, :], b_, b_.ap, ALU.add)
                STO(m_, MODV[l, :, j * 512:(j + 1) * 512], m_.ap)
        em.barrier()
        chk()
        AR.reset()

        def mod_tiles(l, v, which, A_t, B_t, tmp_t):
            base = (l * 3 + v) * 6 * D + which * 3 * D
            LD(B_t, B_t.ap, bc_row(base, MODV, D))
            LD(tmp_t, tmp_t.ap, bc_row(base + D, MODV, D))
            nrm = I["norm1"] if which == 0 else I["norm2"]
            LD(A_t, A_t.ap, bc_row(l * D, nrm, D))
            V("dve", lambda g: g.scalar_tensor_tensor(out=A_t.ap, in0=tmp_t.ap, scalar=1.0, in1=A_t.ap, op0=ALU.add,
                                                      op1=ALU.mult), r=[tmp_t, A_t], w=[A_t])

        def gate_tile(l, v, which, G_t):
            base = (l * 3 + v) * 6 * D + which * 3 * D + 2 * D
            LD(G_t, G_t.ap, bc_row(base, MODV, D))

        def rms_scale(x_t, stat_t, junk_t, width, eps, ncols=1):
            V("dve", lambda g: g.memset(stat_t.ap[:, 0:1], 0.0), w=[stat_t])
            V("act", lambda g: g.activation(out=junk_t.ap, in_=x_t.ap, func=AF.Square, accum_out=stat_t.ap[:, 0:1]),
              r=[x_t, stat_t], w=[junk_t, stat_t])
            V("dve", lambda g: g.tensor_scalar(out=stat_t.ap[:, 0:1], in0=stat_t.ap[:, 0:1], scalar1=1.0 / width,
                                               scalar2=eps, op0=ALU.mult, op1=ALU.add), r=[stat_t], w=[stat_t])
            V("act", lambda g: g.activation(out=stat_t.ap[:, 0:1], in_=stat_t.ap[:, 0:1], func=AF.Sqrt), r=[stat_t], w=[stat_t])
            V("dve", lambda g: g.reciprocal(out=stat_t.ap[:, 0:1], in_=stat_t.ap[:, 0:1]), r=[stat_t], w=[stat_t])

        def streams(b):
            return [(2, 0, NTC), (b, NTC, NTL)]

        for l in range(L):
            HT = AR.alloc([128, 8, NB * NT], BF16, "HT")
            A_t = AR.alloc([128, D], F32, "A1")
            B_t = AR.alloc([128, D], F32, "B1")
            tmpm = AR.alloc([128, D], F32, "tmpm")
            xts = [AR.alloc([128, D], F32, f"xt{i}") for i in range(2)]
            junk = AR.alloc([128, D], F32, "junk")
            hbs = [AR.alloc([128, D], BF16, f"hb{i}") for i in range(2)]
            stats = [AR.alloc([128, 2], F32, f"st{i}") for i in range(2)]
            it = 0
            for b in range(NB):
                for (v, t0, ntl) in streams(b):
                    mod_tiles(l, v, 0, A_t, B_t, tmpm)
                    dbg('A1', A_t, [128, D])
                    dbg('B1', B_t, [128, D])
                    for i in range(t0, t0 + ntl):
                        xt, hb, stt_ = xts[it % 2], hbs[it % 2], stats[it % 2]
                        it += 1
                        LD(xt, xt.ap, XS[b, i * 128:(i + 1) * 128, :])
                        dbg('xt0', xt, [128, D])
                        rms_scale(xt, stt_, junk, D, 1e-6)
                        dbg('st0', stt_, [128, 2])
                        V("dve", lambda g, xt=xt, stt_=stt_: g.scalar_tensor_tensor(out=xt.ap, in0=xt.ap, scalar=stt_.ap[:, 0:1],
                                                                                    in1=A_t.ap, op0=ALU.mult, op1=ALU.mult),
                          r=[xt, stt_, A_t], w=[xt])
                        dbg('xt1', xt, [128, D])
                        tt("dve", hb, hb.ap, xt, xt.ap, B_t, B_t.ap, ALU.add)
                        dbg('hb', hb, [128, D], BF16)
                        p = psum()
                        pb = bfv(p)
                        for k in range(8):
                            V("pe", lambda g, k=k, hb=hb, pb=pb: g.transpose(out=pb[:, k * 128:(k + 1) * 128],
                                                                             in_=hb.ap[:, k * 128:(k + 1) * 128], identity=ident_b),
                              r=[hb, cstb], w=[p])
                        col = b * NT + i * 128
                        V("act", lambda g, pb=pb, col=col: g.copy(out=HT.ap[:, :, col:col + 128],
                                                                  in_=pb.rearrange("p (k t) -> p k t", k=8)), r=[p], w=[HT])
            WBLK = 512
            wbl = [AR.alloc([128, 8, WBLK], BF16, f"wbl{i}") for i in range(2)]
            zst = [AR.alloc([128, WBLK], F32, f"zst{i}") for i in range(3)]
            it = 0
            wblocks = [(c0, min(WBLK, INW - c0)) for c0 in range(0, INW, WBLK)]
            for j, (c0, cw_) in enumerate(wblocks):
                w_ = wbl[j % 2]
                LD(w_, w_.ap[:, :, 0:cw_], I["w_in"][l, :, c0:c0 + cw_].rearrange("(k p) n -> p k n", p=128), q="pool")
                for b in range(NB):
                    for i in range(NTT):
                        p = psum()
                        col = b * NT + i * 128
                        for k in range(8):
                            V("pe", lambda g, k=k, p=p, w_=w_, col=col, cw_=cw_: g.matmul(p.ap[:, 0:cw_], lhsT=HT.ap[:, k, col:col + 128],
                                                                                        rhs=w_.ap[:, k, 0:cw_], start=(k == 0), stop=(k == 7)),
                              r=[HT, w_], w=[p])
                        z_ = zst[it % 3]
                        eng = "act" if it % 2 == 0 else "dve"
                        it += 1
                        if eng == "act":
                            V("act", lambda g, z_=z_, p=p, cw_=cw_: g.copy(out=z_.ap[:, 0:cw_], in_=p.ap[:, 0:cw_]), r=[p], w=[z_])
                        else:
                            V("dve", lambda g, z_=z_, p=p, cw_=cw_: g.tensor_copy(out=z_.ap[:, 0:cw_], in_=p.ap[:, 0:cw_]), r=[p], w=[z_])
                        r0 = zrow(i * 128)
                        STO(z_, ZIN[b, r0:r0 + 128, c0:c0 + cw_], z_.ap[:, 0:cw_], q="sp")
            em.barrier()
            chk()
            AR.reset()

            gq = AR.alloc([128, 10, 64], F32, "gq")
            LD(gq, gq.ap[:, 0:8, :], dap(I["q_gain"], l * 64, [[0, 128], [0, 8], [1, 64]]))
            LD(gq, gq.ap[:, 8:10, :], dap(I["k_gain"], l * 64, [[0, 128], [0, 2], [1, 64]]))
            qT = AR.alloc([64, 8, NT], BF16, "qT")
            kT = AR.alloc([64, 2, NT], BF16, "kT")
            vA = AR.alloc([128, NTT, 2, 65], BF16, "vA")
            zts = [AR.alloc([128, 768], F32, f"zt{i}") for i in range(2)]
            sq = AR.alloc([128, 640], F32, "sq")
            ss = AR.alloc([128, 10], F32, "ss")
            qk = AR.alloc([128, 10, 64], F32, "qk")
            r1 = AR.alloc([128, 10, 2, 16], F32, "r1")
            r2 = AR.alloc([128, 10, 2, 16], F32, "r2")
            qkb = AR.alloc([128, 10, 64], BF16, "qkb")
            pTs = [AR.alloc([128, QB], BF16, f"pT{i}") for i in range(2)]
            oatt = AR.alloc([128, QB // 128, 512], F32, "oatt")
            rec = AR.alloc([128, 4, 1], F32, "rec")
            for b in range(NB):
                V("dve", lambda g: g.memset(vA.ap[:, :, :, 64:65], 1.0), w=[vA])
                for i in range(NTT):
                    z = zts[i % 2]
                    r0 = zrow(i * 128)
                    LD(z, z.ap, ZIN[b, r0:r0 + 128, 0:768])
                    V("act", lambda g, z=z: g.activation(out=sq.ap, in_=z.ap[:, 0:640], func=AF.Square), r=[z], w=[sq])
                    V("dve", lambda g: g.tensor_reduce(out=ss.ap, in_=sq.ap.rearrange("p (h d) -> p h d", d=64), axis=AX.X,
                                                       op=ALU.add), r=[sq], w=[ss])
                    V("dve", lambda g: g.tensor_scalar(out=ss.ap, in0=ss.ap, scalar1=1.0 / 64, scalar2=1e-6, op0=ALU.mult,
                                                       op1=ALU.add), r=[ss], w=[ss])
                    V("act", lambda g: g.activation(out=ss.ap, in_=ss.ap, func=AF.Sqrt), r=[ss], w=[ss])
                    V("dve", lambda g: g.reciprocal(out=ss.ap, in_=ss.ap), r=[ss], w=[ss])
                    V("dve", lambda g, z=z: g.tensor_tensor(out=qk.ap, in0=z.ap[:, 0:640].rearrange("p (h d) -> p h d", d=64),
                                                            in1=ss.ap.unsqueeze(2).broadcast_to([128, 10, 64]), op=ALU.mult),
                      r=[z, ss], w=[qk])
                    tt("dve", qk, qk.ap, qk, qk.ap, gq, gq.ap, ALU.mult)
                    if i >= NTC:
                        il = i - NTC
                        q5 = qk.ap.rearrange("p h (a x f) -> p h a x f", a=2, x=2)
                        o5 = qkb.ap.rearrange("p h (a x f) -> p h a x f", a=2, x=2)
                        x1, x2 = q5[:, :, :, 0, :], q5[:, :, :, 1, :]
                        cos = rope.ap[:, il, 0:32].rearrange("p (a f) -> p a f", a=2).unsqueeze(1).broadcast_to([128, 10, 2, 16])
                        sin = rope.ap[:, il, 32:64].rearrange("p (a f) -> p a f", a=2).unsqueeze(1).broadcast_to([128, 10, 2, 16])
                        tt("dve", r1, r1.ap, qk, x1, rope, cos, ALU.mult)
                        tt("dve", r2, r2.ap, qk, x2, rope, sin, ALU.mult)
                        tt("dve", qkb, o5[:, :, :, 0, :], r1, r1.ap, r2, r2.ap, ALU.subtract)
                        tt("dve", r1, r1.ap, qk, x1, rope, sin, ALU.mult)
                        tt("dve", r2, r2.ap, qk, x2, rope, cos, ALU.mult)
                        tt("dve", qkb, o5[:, :, :, 1, :], r1, r1.ap, r2, r2.ap, ALU.add)
                    else:
                        V("dve", lambda g: g.tensor_copy(out=qkb.ap, in_=qk.ap), r=[qk], w=[qkb])
                    V("act", lambda g, z=z, i=i: g.copy(out=vA.ap[:, i, :, 0:64], in_=z.ap[:, 640:768].rearrange("p (h d) -> p h d", d=64)),
                      r=[z], w=[vA])
                    p1, p2 = psum(), psum()
                    pb1, pb2 = bfv(p1), bfv(p2)
                    for h in range(10):
                        dst = pb1[0:64, h * 128:(h + 1) * 128] if h < 8 else pb2[0:64, (h - 8) * 128:(h - 7) * 128]
                        V("pe", lambda g, h=h, dst=dst: g.transpose(out=dst, in_=qkb.ap[:, h, :], identity=ident_b),
                          r=[qkb, cstb], w=[p1 if h < 8 else p2])
                    V("act", lambda g, pb1=pb1, i=i: g.copy(out=qT.ap[:, :, i * 128:(i + 1) * 128],
                                                            in_=pb1[0:64, :].rearrange("p (h t) -> p h t", h=8)), r=[p1], w=[qT])
                    V("dve", lambda g, pb2=pb2, i=i: g.tensor_copy(out=kT.ap[:, :, i * 128:(i + 1) * 128],
                                                                   in_=pb2[0:64, 0:256].rearrange("p (h t) -> p h t", h=2)), r=[p2], w=[kT])
                blocks = [(0, C, NTC)] + [(C + qb * QB, QB, NTT) for qb in range(T // QB)]
                itp = 0
                for (q0, qn, nkt) in blocks:
                    nsub = qn // 128
                    for h in range(8):
                        kvh = h // 4
                        for s in range(nkt):
                            p = psum()
                            V("pe", lambda g, p=p, s=s, h=h, kvh=kvh, q0=q0, qn=qn: g.matmul(
                                p.ap[:, 0:qn], lhsT=kT.ap[:, kvh, s * 128:(s + 1) * 128], rhs=qT.ap[:, h, q0:q0 + qn],
                                start=True, stop=True), r=[kT, qT], w=[p])
                            pT = pTs[itp % 2]
                            itp += 1
                            V("act", lambda g, p=p, pT=pT, qn=qn: g.activation(out=pT.ap[:, 0:qn], in_=p.ap[:, 0:qn], func=AF.Exp,
                                                                               scale=0.125), r=[p], w=[pT])
                            for j in range(nsub):
                                V("pe", lambda g, j=j, s=s, pT=pT, kvh=kvh, nkt=nkt: g.matmul(
                                    PS[j].ap[:, 0:65], lhsT=pT.ap[:, j * 128:(j + 1) * 128], rhs=vA.ap[:, s, kvh, :],
                                    start=(s == 0), stop=(s == nkt - 1)), r=[pT, vA], w=[PS[j]])
                        for j in range(nsub):
                            V("dve", lambda g, j=j: g.reciprocal(out=rec.ap[:, j, :], in_=PS[j].ap[:, 64:65]), r=[PS[j]], w=[rec])
                            V("dve", lambda g, j=j, h=h: g.tensor_scalar(out=oatt.ap[:, j, h * 64:(h + 1) * 64], in0=PS[j].ap[:, 0:64], scalar1=rec.ap[:, j, :],
                                                                         scalar2=None, op0=ALU.mult), r=[PS[j], rec], w=[oatt])
                    for j in range(nsub):
                        n0 = q0 + j * 128
                        STO(oatt, OBR[b, n0:n0 + 128, 0, :], oatt.ap[:, j, :])
            em.barrier()
            chk()
            AR.reset()

            mu = AR.alloc([128, 1920], F32, "mu")
            LD(mu, mu.ap, bc_row(l * 1920, I["shift_mu"], 1920))
            w0 = AR.alloc([128, 2, 512], F32, "w0")
            LD(w0, w0.ap, bc_row(l * 1024, I["decay_w0"], 1024).rearrange("p (d c) -> p d c", d=2))
            a0 = AR.alloc([128, 2, 512], F32, "a0")
            LD(a0, a0.ap, bc_row(l * 1024, I["iclr_a0"], 1024).rearrange("p (d c) -> p d c", d=2))
            pkk = AR.alloc([128, 512], F32, "pkk")
            LD(pkk, pkk.ap, bc_row(l * 512, I["rwkv_kk"], 512))
            pka = AR.alloc([128, 512], F32, "pka")
            LD(pka, pka.ap, bc_row(l * 512, I["rwkv_ka"], 512))
            prk = AR.alloc([128, 512], F32, "prk")
            LD(prk, prk.ap, bc_row(l * 512, I["rwkv_rk"], 512))
            w2 = AR.alloc([64, 2, 512], BF16, "w2")
            LD(w2, w2.ap, I["decay_w2"][l].rearrange("d r c -> r d c"), q="pool")
            a2 = AR.alloc([64, 2, 512], BF16, "a2")
            LD(a2, a2.ap, I["iclr_a2"][l].rearrange("d r c -> r d c"), q="pool")
            g2w = AR.alloc([128, 512], BF16, "g2w")
            LD(g2w, g2w.ap, I["gate_g2"][l], q="pool")
            curs = [AR.alloc([128, 1920], F32, f"cur{i}") for i in range(2)]
            prvs = [AR.alloc([128, 1920], F32, f"prv{i}") for i in range(2)]
            nxts = [AR.alloc([128, 1920], F32, f"nxt{i}") for i in range(2)]
            lr_2 = [AR.alloc([128, 384], BF16, f"lr{i}") for i in range(2)]
            lrT_2 = [AR.alloc([128, 5, 128], BF16, f"lrT{i}") for i in range(2)]
            tA_2 = [AR.alloc([128, 512], F32, f"tA{i}") for i in range(2)]
            tB_2 = [AR.alloc([128, 512], F32, f"tB{i}") for i in range(2)]
            kkt_2 = [AR.alloc([128, 512], F32, f"kkt{i}") for i in range(2)]
            s8_2 = [AR.alloc([128, 8], F32, f"s8{i}") for i in range(2)]
            ad_2 = [[AR.alloc([128, 512], F32, f"ad{d}{i}") for d in range(2)] for i in range(2)]
            sts_2 = [[AR.alloc([128, 8, 6, 64], F32, f"sis{d}{i}") for d in range(2)] for i in range(2)]
            stf_2 = [AR.alloc([128, 8 * 6 * 64], F32, f"stf{i}") for i in range(2)]
            aux_2 = [AR.alloc([128, 1024], F32, f"aux{i}") for i in range(2)]

            def v3(ap2d):
                return ap2d.rearrange("p (h d) -> p h d", d=64)

            it = 0
            for b in range(NB):
                for (v, t0, ntl) in streams(b):
                    seg0, seg1 = t0 * 128, (t0 + ntl) * 128
                    for i in range(t0, t0 + ntl):
                        cur, prv, nxt = curs[it % 2], prvs[it % 2], nxts[it % 2]
                        par = it % 2
                        lr, lrT, tA, tB, kkt, s8, stf, aux = [x[par] for x in (lr_2, lrT_2, tA_2, tB_2, kkt_2, s8_2, stf_2, aux_2)]
                        ad, sts = ad_2[par], sts_2[par]
                        it += 1
                        r0 = zrow(i * 128)
                        LD(cur, cur.ap, ZIN[b, r0:r0 + 128, O_RW:O_RW + 1920])
                        LD(prv, prv.ap, ZIN[b, r0 - 1:r0 + 127, O_RW:O_RW + 1920])
                        LD(nxt, nxt.ap, ZIN[b, r0 + 1:r0 + 129, O_RW:O_RW + 1920])
                        tt("dve", prv, prv.ap, prv, prv.ap, nxt, nxt.ap, ALU.add)
                        V("dve", lambda g, prv=prv, cur=cur: g.scalar_tensor_tensor(out=prv.ap, in0=prv.ap, scalar=0.5, in1=cur.ap,
                                                                                    op0=ALU.mult, op1=ALU.subtract), r=[prv, cur], w=[prv])
                        tt("dve", prv, prv.ap, prv, prv.ap, mu, mu.ap, ALU.mult)
                        tt("dve", cur, cur.ap, prv, prv.ap, cur, cur.ap, ALU.add)
                        seg = cur
                        R_, K_, V_ = seg.ap[:, 0:512], seg.ap[:, 512:1024], seg.ap[:, 1024:1536]
                        V("act", lambda g, seg=seg: g.activation(out=lr.ap[:, 0:128], in_=seg.ap[:, 1536:1664], func=AF.Tanh), r=[seg], w=[lr])
                        V("dve", lambda g, seg=seg: g.tensor_copy(out=lr.ap[:, 128:256], in_=seg.ap[:, 1664:1792]), r=[seg], w=[lr])
                        V("act", lambda g, seg=seg: g.activation(out=lr.ap[:, 256:384], in_=seg.ap[:, 1792:1920], func=AF.Sigmoid), r=[seg], w=[lr])
                        p = psum()
                        pb = bfv(p)
                        for k in range(4):
                            V("pe", lambda g, k=k, pb=pb: g.transpose(out=pb[0:64, k * 128:(k + 1) * 128], in_=lr.ap[:, k * 64:(k + 1) * 64],
                                                                      identity=ident_b), r=[lr, cstb], w=[p])
                        V("pe", lambda g, pb=pb: g.transpose(out=pb[:, 512:640], in_=lr.ap[:, 256:384], identity=ident_b), r=[lr, cstb], w=[p])
                        V("dve", lambda g, pb=pb: g.tensor_copy(out=lrT.ap[0:64, 0:4, :], in_=pb[0:64, 0:512].rearrange("p (k t) -> p k t", k=4)), r=[p], w=[lrT])
                        V("dve", lambda g, pb=pb: g.tensor_copy(out=lrT.ap[:, 4, :], in_=pb[:, 512:640]), r=[p], w=[lrT])
                        tt("dve", kkt, kkt.ap, seg, K_, pkk, pkk.ap, ALU.mult)
                        V("act", lambda g: g.activation(out=tA.ap, in_=kkt.ap, func=AF.Square), r=[kkt], w=[tA])
                        V("dve", lambda g: g.tensor_reduce(out=s8.ap, in_=v3(tA.ap), axis=AX.X, op=ALU.add), r=[tA], w=[s8])
                        V("dve", lambda g: g.tensor_scalar(out=s8.ap, in0=s8.ap, scalar1=1e-12, scalar2=None, op0=ALU.add), r=[s8], w=[s8])
                        V("act", lambda g: g.activation(out=s8.ap, in_=s8.ap, func=AF.Sqrt), r=[s8], w=[s8])
                        V("dve", lambda g: g.reciprocal(out=s8.ap, in_=s8.ap), r=[s8], w=[s8])
                        V("dve", lambda g: g.tensor_tensor(out=v3(kkt.ap), in0=v3(kkt.ap), in1=s8.ap.unsqueeze(2).broadcast_to([128, 8, 64]),
                                                           op=ALU.mult), r=[kkt, s8], w=[kkt])
                        for d in range(2):
                            sd = sts[d]
                            lo, hi = d * 64, (d + 1) * 64
                            p = psum()
                            V("pe", lambda g, p=p, d=d: g.matmul(p.ap, lhsT=lrT.ap[0:64, d, :], rhs=w2.ap[:, d, :], start=True, stop=True),
                              r=[lrT, w2], w=[p])
                            tt("dve", tA, tA.ap, p, p.ap, w0, w0.ap[:, d, :], ALU.add)
                            V("act", lambda g: g.activation(out=tA.ap, in_=tA.ap, func=AF.Sigmoid), r=[tA], w=[tA])
                            V("act", lambda g, sd=sd: g.activation(out=sd.ap[:, :, 1, :], in_=v3(tA.ap), func=AF.Exp, scale=DECAY_C), r=[tA], w=[sd])
                            p = psum()
                            V("pe", lambda g, p=p, d=d: g.matmul(p.ap, lhsT=lrT.ap[0:64, 2 + d, :], rhs=a2.ap[:, d, :], start=True, stop=True),
                              r=[lrT, a2], w=[p])
                            a_ = ad[d]
                            tt("dve", a_, a_.ap, p, p.ap, a0, a0.ap[:, d, :], ALU.add)
                            V("act", lambda g, a_=a_: g.activation(out=a_.ap, in_=a_.ap, func=AF.Sigmoid), r=[a_], w=[a_])
                            V("dve", lambda g, a_=a_: g.scalar_tensor_tensor(out=tB.ap, in0=a_.ap, scalar=-1.0, in1=pka.ap, op0=ALU.add, op1=ALU.mult),
                              r=[a_, pka], w=[tB])
                            V("dve", lambda g, sd=sd, K_=K_: g.scalar_tensor_tensor(out=sd.ap[:, :, 3, :], in0=v3(tB.ap), scalar=1.0, in1=v3(K_),
                                                                                    op0=ALU.add, op1=ALU.mult), r=[tB, seg], w=[sd])
                            V("dve", lambda g, sd=sd, a_=a_: g.tensor_tensor(out=sd.ap[:, :, 2, :], in0=v3(kkt.ap), in1=v3(a_.ap), op=ALU.mult),
                              r=[kkt, a_], w=[sd])
                            V("dve", lambda g, sd=sd: g.tensor_scalar(out=sd.ap[:, :, 0, :], in0=v3(kkt.ap), scalar1=-1.0, scalar2=None, op0=ALU.mult),
                              r=[kkt], w=[sd])
                            V("act", lambda g, sd=sd, R_=R_: g.copy(out=sd.ap[:, :, 4, :], in_=v3(R_)), r=[seg], w=[sd])
                            V("act", lambda g, sd=sd, V_=V_: g.copy(out=sd.ap[:, :, 5, :], in_=v3(V_)), r=[seg], w=[sd])
                        p = psum()
                        V("pe", lambda g, p=p: g.matmul(p.ap, lhsT=lrT.ap[:, 4, :], rhs=g2w.ap, start=True, stop=True), r=[lrT, g2w], w=[p])
                        V("act", lambda g, p=p: g.copy(out=aux.ap[:, 512:1024], in_=p.ap), r=[p], w=[aux])
                        tt("dve", tA, tA.ap, seg, R_, seg, K_, ALU.mult)
                        tt("dve", tA, tA.ap, tA, tA.ap, prk, prk.ap, ALU.mult)
                        V("dve", lambda g: g.tensor_reduce(out=s8.ap, in_=v3(tA.ap), axis=AX.X, op=ALU.add), r=[tA], w=[s8])
                        V("dve", lambda g, V_=V_: g.tensor_tensor(out=v3(aux.ap[:, 0:512]), in0=v3(V_), in1=s8.ap.unsqueeze(2).broadcast_to([128, 8, 64]),
                                                                  op=ALU.mult), r=[seg, s8], w=[aux])
                        STO(aux, RWAUX[b, i * 128:(i + 1) * 128, :], aux.ap)
                        ch0 = (b * 2 + 0) * 8
                        STO(sts[0], dap(SI, (ch0 * NT + i * 128) * 384, [[384, 128], [NT * 384, 8], [1, 384]]),
                            sts[0].ap.rearrange("p h s d -> p h (s d)"))
                        s1f = sts[1].ap.rearrange("p h s d -> p (h s d)")
                        for cblk in range(6):
                            p = psum()
                            V("pe", lambda g, p=p, cblk=cblk, s1f=s1f: g.matmul(p.ap, lhsT=Jm, rhs=s1f[:, cblk * 512:(cblk + 1) * 512], start=True, stop=True),
                              r=[sts[1], cst], w=[p])
                            if cblk % 2 == 0:
                                V("act", lambda g, p=p, cblk=cblk: g.copy(out=stf.ap[:, cblk * 512:(cblk + 1) * 512], in_=p.ap), r=[p], w=[stf])
                            else:
                                V("dve", lambda g, p=p, cblk=cblk: g.tensor_copy(out=stf.ap[:, cblk * 512:(cblk + 1) * 512], in_=p.ap), r=[p], w=[stf])
                        ch1 = (b * 2 + 1) * 8
                        s0 = seg0 + seg1 - 128 - i * 128
                        STO(stf, dap(SI, (ch1 * NT + s0) * 384, [[384, 128], [NT * 384, 8], [1, 384]]),
                            stf.ap.rearrange("p (h x) -> p h x", h=8))
            em.barrier()
            chk()
            AR.reset()

            CH = 16
            S_ = AR.alloc([128, 16, 64], F32, "S")
            T_ = AR.alloc([128, 16, 64], F32, "Tt")
            U_ = AR.alloc([128, 2, 16, 64], F32, "U")
            sic = [AR.alloc([128, CH, 5, 64], F32, f"sic{i}") for i in range(2)]
            vbc = [AR.alloc([128, CH, 2, 16], F32, f"vbc{i}") for i in range(2)]
            yoc = [AR.alloc([128, CH, 16], F32, f"yoc{i}") for i in range(2)]
            V("dve", lambda g: g.memset(S_.ap, 0.0), w=[S_])
            NCH = NB * 16
            for c in range(NT // CH):
                si, vb, yo = sic[c % 2], vbc[c % 2], yoc[c % 2]
                s0 = c * CH
                for vq in range(4):
                    em.dma("sp", si.ap[vq * 32:vq * 32 + NCH], dap(SI, s0 * 384, [[NT * 384, NCH], [384, CH], [1, 320]]).rearrange("c s (a k) -> c s a k", a=5),
                           writes=[si.b])
                    em.dma("sp", vb.ap[vq * 32:vq * 32 + NCH, :, 1, :], dap(SI, s0 * 384 + 320 + vq * 16, [[NT * 384, NCH], [384, CH], [1, 16]]),
                           writes=[vb.b])
                for s in range(CH):
                    A_b = si.ap[:, s, 0, :].unsqueeze(1).broadcast_to([128, 16, 64])
                    W_b = si.ap[:, s, 1, :].unsqueeze(1).broadcast_to([128, 16, 64])
                    R_b = si.ap[:, s, 4, :].unsqueeze(1).broadcast_to([128, 16, 64])
                    BK = si.ap[:, s, 2:4, :].unsqueeze(2).broadcast_to([128, 2, 16, 64])
                    SAV = vb.ap[:, s, :, :].unsqueeze(3).broadcast_to([128, 2, 16, 64])
                    tt("dve", T_, T_.ap, S_, S_.ap, si, A_b, ALU.mult)
                    V("dve", lambda g, vb=vb, s=s: g.tensor_reduce(out=vb.ap[:, s, 0, :], in_=T_.ap, axis=AX.X, op=ALU.add), r=[T_], w=[vb])
                    tt("dve", S_, S_.ap, S_, S_.ap, si, W_b, ALU.mult)
                    V("dve", lambda g, SAV=SAV, BK=BK: g.tensor_tensor(out=U_.ap, in0=SAV, in1=BK, op=ALU.mult), r=[vb, si], w=[U_])
                    tt("dve", S_, S_.ap, S_, S_.ap, U_, U_.ap[:, 0], ALU.add)
                    tt("dve", S_, S_.ap, S_, S_.ap, U_, U_.ap[:, 1], ALU.add)
                    tt("dve", T_, T_.ap, S_, S_.ap, si, R_b, ALU.mult)
                    V("dve", lambda g, yo=yo, s=s: g.tensor_reduce(out=yo.ap[:, s, :], in_=T_.ap, axis=AX.X, op=ALU.add), r=[T_], w=[yo])
                for vq in range(4):
                    em.dma("pool", dap(YO, (vq * NCH * NT + s0) * 16, [[NT * 16, NCH], [16, CH], [1, 16]]), yo.ap[vq * 32:vq * 32 + NCH],
                           reads=[yo.b])
            em.barrier()
            chk()
            AR.reset()

            gnw = AR.alloc([128, 512], F32, "gnw")
            LD(gnw, gnw.ap, bc_row(l * 512, I["rwkv_gn_w"], 512))
            gnb = AR.alloc([128, 512], F32, "gnb")
            LD(gnb, gnb.ap, bc_row(l * 512, I["rwkv_gn_b"], 512))
            cw = AR.alloc([128, 3, 512], F32, "cw")
            LD(cw, cw.ap, bc_row(l * 1536, I["conv_w"], 1536).rearrange("p (j c) -> p j c", j=3))
            y0s = [AR.alloc([128, 512], F32, f"y0{i}") for i in range(2)]
            y1s = [AR.alloc([128, 512], F32, f"y1{i}") for i in range(2)]
            axs = [AR.alloc([128, 1024], F32, f"ax{i}") for i in range(2)]
            yy_2 = [AR.alloc([128, 512], F32, f"yy{i}") for i in range(2)]
            ysq_2 = [AR.alloc([128, 512], F32, f"ysq{i}") for i in range(2)]
            m8_2 = [AR.alloc([128, 8], F32, f"m8{i}") for i in range(2)]
            orw = [AR.alloc([128, 512], F32, f"orw{i}") for i in range(2)]
            ccs = [AR.alloc([128, 1536], F32, f"cc{i}") for i in range(2)]
            cps = [AR.alloc([128, 1024], F32, f"cp{i}") for i in range(2)]
            cns = [AR.alloc([128, 1024], F32, f"cn{i}") for i in range(2)]
            ocv = [AR.alloc([128, 512], F32, f"ocv{i}") for i in range(2)]
            it = 0
            for b in range(NB):
                for (v, t0, ntl) in streams(b):
                    seg0, seg1 = t0 * 128, (t0 + ntl) * 128
                    for i in range(t0, t0 + ntl):
                        y0, y1, ax, o_ = y0s[it % 2], y1s[it % 2], axs[it % 2], orw[it % 2]
                        cc, cp, cn, oc = ccs[it % 2], cps[it % 2], cns[it % 2], ocv[it % 2]
                        yy, ysq, m8 = yy_2[it % 2], ysq_2[it % 2], m8_2[it % 2]
                        it += 1
                        ch0, ch1 = (b * 2) * 8, (b * 2 + 1) * 8
                        s1 = seg0 + seg1 - 128 - i * 128
                        for vq in range(4):
                            em.dma("sp", y0.ap.rearrange("p (h q f) -> p h q f", h=8, q=4)[:, :, vq, :],
                                   dap(YO, ((vq * NCH + ch0) * NT + i * 128) * 16, [[16, 128], [NT * 16, 8], [1, 16]]), writes=[y0.b])
                            em.dma("sp", y1.ap.rearrange("p (h q f) -> p h q f", h=8, q=4)[:, :, vq, :],
                                   dap(YO, ((vq * NCH + ch1) * NT + s1) * 16, [[16, 128], [NT * 16, 8], [1, 16]]), writes=[y1.b])
                        LD(ax, ax.ap, RWAUX[b, i * 128:(i + 1) * 128, :])
                        p = psum()
                        V("pe", lambda g, p=p, y1=y1: g.matmul(p.ap, lhsT=Jm, rhs=y1.ap, start=True, stop=True), r=[y1, cst], w=[p])
                        tt("dve", yy, yy.ap, y0, y0.ap, p, p.ap, ALU.add)
                        V("dve", lambda g: g.tensor_reduce(out=m8.ap, in_=v3(yy.ap), axis=AX.X, op=ALU.add), r=[yy], w=[m8])
                        V("dve", lambda g: g.tensor_scalar(out=m8.ap, in0=m8.ap, scalar1=-1.0 / 64, scalar2=None, op0=ALU.mult), r=[m8], w=[m8])
                        V("dve", lambda g: g.tensor_tensor(out=v3(yy.ap), in0=v3(yy.ap), in1=m8.ap.unsqueeze(2).broadcast_to([128, 8, 64]), op=ALU.add),
                          r=[yy, m8], w=[yy])
                        V("act", lambda g: g.activation(out=ysq.ap, in_=yy.ap, func=AF.Square), r=[yy], w=[ysq])
                        V("dve", lambda g: g.tensor_reduce(out=m8.ap, in_=v3(ysq.ap), axis=AX.X, op=ALU.add), r=[ysq], w=[m8])
                        V("dve", lambda g: g.tensor_scalar(out=m8.ap, in0=m8.ap, scalar1=1.0 / 64, scalar2=64e-5, op0=ALU.mult, op1=ALU.add), r=[m8], w=[m8])
                        V("act", lambda g: g.activation(out=m8.ap, in_=m8.ap, func=AF.Sqrt), r=[m8], w=[m8])
                        V("dve", lambda g: g.reciprocal(out=m8.ap, in_=m8.ap), r=[m8], w=[m8])
                        V("dve", lambda g: g.tensor_tensor(out=v3(yy.ap), in0=v3(yy.ap), in1=m8.ap.unsqueeze(2).broadcast_to([128, 8, 64]), op=ALU.mult),
                          r=[yy, m8], w=[yy])
                        tt("dve", yy, yy.ap, yy, yy.ap, gnw, gnw.ap, ALU.mult)
                        tt("dve", yy, yy.ap, yy, yy.ap, gnb, gnb.ap, ALU.add)
                        tt("dve", yy, yy.ap, yy, yy.ap, ax, ax.ap[:, 0:512], ALU.add)
                        tt("dve", o_, o_.ap, yy, yy.ap, ax, ax.ap[:, 512:1024], ALU.mult)
                        STO(o_, OBR[b, i * 128:(i + 1) * 128, 1, :], o_.ap)
                        r0 = zrow(i * 128)
                        LD(cc, cc.ap, ZIN[b, r0:r0 + 128, O_CV:O_CV + 1536])
                        LD(cp, cp.ap, ZIN[b, r0 - 1:r0 + 127, O_CV + 512:O_CV + 1536])
                        LD(cn, cn.ap, ZIN[b, r0 + 1:r0 + 129, O_CV + 512:O_CV + 1536])
                        tt("dve", cc, cc.ap[:, 512:1024], cc, cc.ap[:, 512:1024], cc, cc.ap[:, 1024:1536], ALU.mult)
                        tt("dve", cp, cp.ap[:, 0:512], cp, cp.ap[:, 0:512], cp, cp.ap[:, 512:1024], ALU.mult)
                        tt("dve", cn, cn.ap[:, 0:512], cn, cn.ap[:, 0:512], cn, cn.ap[:, 512:1024], ALU.mult)
                        tt("dve", cc, cc.ap[:, 512:1024], cc, cc.ap[:, 512:1024], cw, cw.ap[:, 1, :], ALU.mult)
                        tt("dve", cp, cp.ap[:, 0:512], cp, cp.ap[:, 0:512], cw, cw.ap[:, 0, :], ALU.mult)
                        tt("dve", cn, cn.ap[:, 0:512], cn, cn.ap[:, 0:512], cw, cw.ap[:, 2, :], ALU.mult)
                        tt("dve", cc, cc.ap[:, 512:1024], cc, cc.ap[:, 512:1024], cp, cp.ap[:, 0:512], ALU.add)
                        tt("dve", cc, cc.ap[:, 512:1024], cc, cc.ap[:, 512:1024], cn, cn.ap[:, 0:512], ALU.add)
                        tt("dve", oc, oc.ap, cc, cc.ap[:, 512:1024], cc, cc.ap[:, 0:512], ALU.mult)
                        STO(oc, OBR[b, i * 128:(i + 1) * 128, 2, :], oc.ap)
            em.barrier()
            chk()
            AR.reset()

            wbr = [AR.alloc([128, 4, D], BF16, f"wbr{i}") for i in range(3)]
            for i_, nm in enumerate(("w_br_att", "w_br_rwkv", "w_br_conv")):
                LD(wbr[i_], wbr[i_].ap, I[nm][l].rearrange("(k p) n -> p k n", p=128), q="pool")
            wo = AR.alloc([128, 8, D], BF16, "wo")
            LD(wo, wo.ap, I["w_out"][l].rearrange("(k p) n -> p k n", p=128), q="pool")
            G1 = AR.alloc([128, D], F32, "G1")
            obs = [AR.alloc([128, 1536], F32, f"ob{i}") for i in range(2)]
            gts = [AR.alloc([128, 3072], F32, f"gt{i}") for i in range(2)]
            xts = [AR.alloc([128, D], F32, f"xm{i}") for i in range(2)]
            obb_2 = [AR.alloc([128, 1536], BF16, f"obb{i}") for i in range(2)]
            oT_2 = [AR.alloc([128, 12, 128], BF16, f"oT{i}") for i in range(2)]
            mm_2 = [AR.alloc([128, D], F32, f"mm{i}") for i in range(2)]
            mt_2 = [AR.alloc([128, 512], F32, f"mt{i}") for i in range(2)]
            mb_2 = [AR.alloc([128, D], BF16, f"mb{i}") for i in range(2)]
            mT_2 = [AR.alloc([128, 8, 128], BF16, f"mT{i}") for i in range(2)]
            xo = [AR.alloc([128, D], F32, f"xo{i}") for i in range(2)]
            it = 0
            for b in range(NB):
                for (v, t0, ntl) in streams(b):
                    gate_tile(l, v, 0, G1)
                    for i in range(t0, t0 + ntl):
                        ob, gt, xt, xo_ = obs[it % 2], gts[it % 2], xts[it % 2], xo[it % 2]
                        obb, oT, mm, mt, mb, mT = [x[it % 2] for x in (obb_2, oT_2, mm_2, mt_2, mb_2, mT_2)]
                        it += 1
                        r0 = zrow(i * 128)
                        LD(ob, ob.ap, OBR[b, i * 128:(i + 1) * 128, :, :].rearrange("p a c -> p (a c)"))
                        LD(gt, gt.ap, ZIN[b, r0:r0 + 128, O_GT:O_GT + 3072])
                        LD(xt, xt.ap, XS[b, i * 128:(i + 1) * 128, :])
                        V("dve", lambda g, ob=ob: g.tensor_copy(out=obb.ap, in_=ob.ap), r=[ob], w=[obb])
                        V("act", lambda g, gt=gt: g.activation(out=gt.ap, in_=gt.ap, func=AF.Sigmoid), r=[gt], w=[gt])
                        p1, p2 = psum(), psum()
                        pb1, pb2 = bfv(p1), bfv(p2)
                        for k in range(12):
                            dst = pb1[:, k * 128:(k + 1) * 128] if k < 8 else pb2[:, (k - 8) * 128:(k - 7) * 128]
                            V("pe", lambda g, k=k, dst=dst: g.transpose(out=dst, in_=obb.ap[:, k * 128:(k + 1) * 128], identity=ident_b),
                              r=[obb, cstb], w=[p1 if k < 8 else p2])
                        V("act", lambda g, pb1=pb1: g.copy(out=oT.ap[:, 0:8, :], in_=pb1.rearrange("p (k t) -> p k t", k=8)), r=[p1], w=[oT])
                        V("dve", lambda g, pb2=pb2: g.tensor_copy(out=oT.ap[:, 8:12, :], in_=pb2[:, 0:512].rearrange("p (k t) -> p k t", k=4)), r=[p2], w=[oT])
                        for br in range(3):
                            for hf in range(2):
                                p = psum()
                                for k in range(4):
                                    V("pe", lambda g, p=p, k=k, br=br, hf=hf: g.matmul(p.ap, lhsT=oT.ap[:, br * 4 + k, :],
                                                                                       rhs=wbr[br].ap[:, k, hf * 512:(hf + 1) * 512],
                                                                                       start=(k == 0), stop=(k == 3)), r=[oT, wbr[br]], w=[p])
                                gsl = gt.ap[:, br * 1024 + hf * 512:br * 1024 + (hf + 1) * 512]
                                if br == 0:
                                    tt("dve", mm, mm.ap[:, hf * 512:(hf + 1) * 512], p, p.ap, gt, gsl, ALU.mult)
                                else:
                                    tt("dve", mt, mt.ap, p, p.ap, gt, gsl, ALU.mult)
                                    tt("dve", mm, mm.ap[:, hf * 512:(hf + 1) * 512], mm, mm.ap[:, hf * 512:(hf + 1) * 512], mt, mt.ap, ALU.add)
                        V("act", lambda g: g.copy(out=mb.ap, in_=mm.ap), r=[mm], w=[mb])
                        p = psum()
                        pb = bfv(p)
                        for k in range(8):
                            V("pe", lambda g, k=k, pb=pb: g.transpose(out=pb[:, k * 128:(k + 1) * 128], in_=mb.ap[:, k * 128:(k + 1) * 128], identity=ident_b),
                              r=[mb, cstb], w=[p])
                        V("act", lambda g, pb=pb: g.copy(out=mT.ap, in_=pb.rearrange("p (k t) -> p k t", k=8)), r=[p], w=[mT])
                        for hf in range(2):
                            p = psum()
                            for k in range(8):
                                V("pe", lambda g, p=p, k=k, hf=hf: g.matmul(p.ap, lhsT=mT.ap[:, k, :], rhs=wo.ap[:, k, hf * 512:(hf + 1) * 512],
                                                                            start=(k == 0), stop=(k == 7)), r=[mT, wo], w=[p])
                            tt("dve", mt, mt.ap, p, p.ap, G1, G1.ap[:, hf * 512:(hf + 1) * 512], ALU.mult)
                            tt("dve", xo_, xo_.ap[:, hf * 512:(hf + 1) * 512], mt, mt.ap, xt, xt.ap[:, hf * 512:(hf + 1) * 512], ALU.add)
                        STO(xo_, XS[b, i * 128:(i + 1) * 128, :], xo_.ap)
            em.barrier()
            chk()
            AR.reset()

            for b in range(NB):
                H2 = AR.alloc([128, NTT, D], BF16, "H2")
                AFF = AR.alloc([128, NTT, NE], F32, "AFF")
                MSK = AR.alloc([128, NTT, NE], F32, "MSK")
                RNK = AR.alloc([128, NTT, NE], F32, "RNK")
                GW = AR.alloc([128, NTT, NE], F32, "GW")
                wr = AR.alloc([128, 8, NE], F32, "wr")
                LD(wr, wr.ap, I["w_router"][l].rearrange("(k p) e -> p k e", p=128))
                AR_mid = AR.off
                A_t = AR.alloc([128, D], F32, "A2")
                B_t = AR.alloc([128, D], F32, "B2")
                tmpm = AR.alloc([128, D], F32, "tmpm2")
                xts = [AR.alloc([128, D], F32, f"xn{i}") for i in range(2)]
                junk = AR.alloc([128, D], F32, "junk2")
                h2f = AR.alloc([128, D], F32, "h2f")
                h2T = AR.alloc([128, 8, 128], F32, "h2T")
                stats = [AR.alloc([128, 2], F32, f"sn{i}") for i in range(2)]
                lg = AR.alloc([128, NE], F32, "lg")
                mx = AR.alloc([128, 2], F32, "mx")
                affT = AR.alloc([NE, max(T, C)], F32, "affT")
                mkT = AR.alloc([NE, max(T, C)], F32, "mkT")
                bj = AR.alloc([NE, max(T, C)], F32, "bj")
                bis = AR.alloc([NE, 8], F32, "bis")
                mskb = AR.alloc([128, NTT, NE], BF16, "mskb")
                tot = AR.alloc([128, NTT, NE], F32, "tot")
                pre = AR.alloc([128, NTT, NE], F32, "pre")
                it = 0
                for (v, t0, ntl) in streams(b):
                    mod_tiles(l, v, 1, A_t, B_t, tmpm)
                    for i in range(t0, t0 + ntl):
                        xt, stt_ = xts[it % 2], stats[it % 2]
                        it += 1
                        LD(xt, xt.ap, XS[b, i * 128:(i + 1) * 128, :])
                        rms_scale(xt, stt_, junk, D, 1e-6)
                        V("dve", lambda g, xt=xt, stt_=stt_: g.scalar_tensor_tensor(out=xt.ap, in0=xt.ap, scalar=stt_.ap[:, 0:1], in1=A_t.ap,
                                                                                    op0=ALU.mult, op1=ALU.mult), r=[xt, stt_, A_t], w=[xt])
                        tt("dve", h2f, h2f.ap, xt, xt.ap, B_t, B_t.ap, ALU.add)
                        V("act", lambda g, i=i: g.copy(out=H2.ap[:, i, :], in_=h2f.ap), r=[h2f], w=[H2])
                        p1, p2 = psum(), psum()
                        for k in range(8):
                            pp = p1 if k < 4 else p2
                            V("pe", lambda g, k=k, pp=pp: g.transpose(out=pp.ap[:, (k % 4) * 128:(k % 4 + 1) * 128], in_=h2f.ap[:, k * 128:(k + 1) * 128],
                                                                      identity=ident), r=[h2f, cst], w=[pp])
                        V("act", lambda g, p1=p1: g.copy(out=h2T.ap[:, 0:4, :], in_=p1.ap.rearrange("p (k t) -> p k t", k=4)), r=[p1], w=[h2T])
                        V("dve", lambda g, p2=p2: g.tensor_copy(out=h2T.ap[:, 4:8, :], in_=p2.ap.rearrange("p (k t) -> p k t", k=4)), r=[p2], w=[h2T])
                        p = psum()
                        for k in range(8):
                            V("pe", lambda g, k=k, p=p: g.matmul(p.ap[:, 0:NE], lhsT=h2T.ap[:, k, :], rhs=wr.ap[:, k, :], start=(k == 0), stop=(k == 7)),
                              r=[h2T, wr], w=[p])
                        V("dve", lambda g, p=p: g.tensor_reduce(out=mx.ap[:, 0:1], in_=p.ap[:, 0:NE], axis=AX.X, op=ALU.max), r=[p], w=[mx])
                        V("dve", lambda g: g.tensor_scalar(out=mx.ap[:, 0:1], in0=mx.ap[:, 0:1], scalar1=-1.0, scalar2=None, op0=ALU.mult), r=[mx], w=[mx])
                        V("dve", lambda g: g.memset(mx.ap[:, 1:2], 0.0), w=[mx])
                        V("act", lambda g, p=p: g.activation(out=lg.ap, in_=p.ap[:, 0:NE], func=AF.Exp, bias=mx.ap[:, 0:1], scale=1.0,
                                                             accum_out=mx.ap[:, 1:2]), r=[p, mx], w=[lg, mx])
                        V("dve", lambda g: g.reciprocal(out=mx.ap[:, 1:2], in_=mx.ap[:, 1:2]), r=[mx], w=[mx])
                        V("dve", lambda g, i=i: g.tensor_scalar(out=AFF.ap[:, i, :], in0=lg.ap, scalar1=mx.ap[:, 1:2], scalar2=None, op0=ALU.mult),
                          r=[lg, mx], w=[AFF])
                stop('8a')
                for (v, t0, ntl) in streams(b):
                    ntok = ntl * 128
                    cap = 2 * ntok // NE
                    for i in range(ntl):
                        if i % 4 == 0:
                            p = psum()
                        V("pe", lambda g, p=p, i=i, t0=t0: g.transpose(out=p.ap[0:NE, (i % 4) * 128:(i % 4 + 1) * 128], in_=AFF.ap[:, t0 + i, :], identity=ident),
                          r=[AFF, cst], w=[p])
                        if i % 4 == 3 or i == ntl - 1:
                            n_ = (i % 4 + 1) * 128
                            c0 = (i // 4) * 512
                            V("dve", lambda g, p=p, n_=n_, c0=c0: g.tensor_copy(out=affT.ap[:, c0:c0 + n_], in_=p.ap[0:NE, 0:n_]), r=[p], w=[affT])
                    stop('8b1_%d' % t0)
                    V("dve", lambda g: g.memset(bis.ap[:, 0:1], 0.0), w=[bis])
                    V("dve", lambda g: g.memset(bis.ap[:, 1:2], 1.0), r=[bis], w=[bis])
                    for _ in range(32):
                        V("dve", lambda g: g.tensor_scalar(out=bis.ap[:, 2:3], in0=bis.ap[:, 0:1], scalar1=bis.ap[:, 1:2], scalar2=0.5, op0=ALU.add, op1=ALU.mult),
                          r=[bis], w=[bis])
                        V("dve", lambda g: g.memset(bis.ap[:, 3:4], 0.0), r=[bis], w=[bis])
                        V("dve", lambda g, ntok=ntok: g.tensor_scalar(out=bj.ap[:, 0:ntok], in0=affT.ap[:, 0:ntok], scalar1=bis.ap[:, 2:3], scalar2=0.0,
                                                                      op0=ALU.is_ge, op1=ALU.add, accum_out=bis.ap[:, 3:4]), r=[affT, bis], w=[bj, bis])
                        V("dve", lambda g, cap=cap: g.tensor_scalar(out=bis.ap[:, 4:5], in0=bis.ap[:, 3:4], scalar1=cap - 0.5, scalar2=None, op0=ALU.is_ge),
                          r=[bis], w=[bis])
                        V("dve", lambda g: g.tensor_tensor(out=bis.ap[:, 5:6], in0=bis.ap[:, 2:3], in1=bis.ap[:, 0:1], op=ALU.subtract), r=[bis], w=[bis])
                        V("dve", lambda g: g.scalar_tensor_tensor(out=bis.ap[:, 0:1], in0=bis.ap[:, 5:6], scalar=bis.ap[:, 4:5], in1=bis.ap[:, 0:1],
                                                                  op0=ALU.mult, op1=ALU.add), r=[bis], w=[bis])
                        V("dve", lambda g: g.tensor_tensor(out=bis.ap[:, 5:6], in0=bis.ap[:, 1:2], in1=bis.ap[:, 2:3], op=ALU.subtract), r=[bis], w=[bis])
                        V("dve", lambda g: g.scalar_tensor_tensor(out=bis.ap[:, 1:2], in0=bis.ap[:, 5:6], scalar=bis.ap[:, 4:5], in1=bis.ap[:, 2:3],
                                                                  op0=ALU.mult, op1=ALU.add), r=[bis], w=[bis])
                    stop('8b2_%d' % t0)
                    V("dve", lambda g, ntok=ntok: g.tensor_scalar(out=mkT.ap[:, 0:ntok], in0=affT.ap[:, 0:ntok], scalar1=bis.ap[:, 0:1], scalar2=None, op0=ALU.is_ge),
                      r=[affT, bis], w=[mkT])
                    p = psum()
                    for i in range(ntl):
                        V("pe", lambda g, p=p, i=i: g.transpose(out=p.ap[:, i * NE:(i + 1) * NE], in_=mkT.ap[0:NE, i * 128:(i + 1) * 128], identity=ident[0:NE, 0:NE]),
                          r=[mkT, cst], w=[p])
                    V("dve", lambda g, p=p, t0=t0, ntl=ntl: g.tensor_copy(out=MSK.ap[:, t0:t0 + ntl, :], in_=p.ap[:, 0:ntl * NE].rearrange("p (i e) -> p i e", e=NE)),
                      r=[p], w=[MSK])
                    V("act", lambda g, t0=t0, ntl=ntl: g.copy(out=mskb.ap[:, t0:t0 + ntl, :], in_=MSK.ap[:, t0:t0 + ntl, :]), r=[MSK], w=[mskb])
                    stop('8b3_%d' % t0)
                    pt_, pu_ = psum(), psum()
                    for i in range(ntl):
                        V("pe", lambda g, i=i, t0=t0, pt_=pt_: g.matmul(pt_.ap[:, i * NE:(i + 1) * NE], lhsT=ones_b, rhs=mskb.ap[:, t0 + i, :], start=True, stop=True),
                          r=[mskb, cstb], w=[pt_])
                        V("pe", lambda g, i=i, t0=t0, pu_=pu_: g.matmul(pu_.ap[:, i * NE:(i + 1) * NE], lhsT=U_b, rhs=mskb.ap[:, t0 + i, :], start=True, stop=True),
                          r=[mskb, cstb], w=[pu_])
                    V("dve", lambda g, pt_=pt_, t0=t0, ntl=ntl: g.tensor_copy(out=tot.ap[:, t0:t0 + ntl, :], in_=pt_.ap[:, 0:ntl * NE].rearrange("p (i e) -> p i e", e=NE)),
                      r=[pt_], w=[tot])
                    V("dve", lambda g, t0=t0: g.memset(pre.ap[:, t0, :], 0.0), w=[pre])
                    for i in range(1, ntl):
                        V("dve", lambda g, i=i, t0=t0: g.tensor_tensor(out=pre.ap[:, t0 + i, :], in0=pre.ap[:, t0 + i - 1, :], in1=tot.ap[:, t0 + i - 1, :], op=ALU.add),
                          r=[pre, tot], w=[pre])
                    V("dve", lambda g, pu_=pu_, t0=t0, ntl=ntl: g.tensor_tensor(out=RNK.ap[:, t0:t0 + ntl, :], in0=pu_.ap[:, 0:ntl * NE].rearrange("p (i e) -> p i e", e=NE),
                                                                                in1=pre.ap[:, t0:t0 + ntl, :], op=ALU.add), r=[pu_, pre], w=[RNK])
                    stop('8b4_%d' % t0)
                stop('8b5')
                tt("dve", GW, GW.ap, AFF, AFF.ap, MSK, MSK.ap, ALU.mult)
                em.barrier()
                chk()
                AR.off = AR_mid
                NU = 12
                wun = [AR.alloc([128, 8, 512], BF16, f"wu{i}") for i in range(NU)]
                Pm = AR.alloc([128, NTT, JL * JW], BF16, "Pm")
                xeT = AR.alloc([128, 8, NS], BF16, "xeT")
                sgt = AR.alloc([128, NS], F32, "sgt")
                hidT = AR.alloc([128, 8, NS], BF16, "hidT")
                yes = [AR.alloc([128, JL + 1, D], BF16, f"yes{i}") for i in range(2)]
                wsrc = (I["exp_gate"], I["exp_up"], I["exp_down"])

                def load_expert(e):
                    for t_ in range(3):
                        for hf in range(2):
                            u = wun[(e % 2) * 6 + t_ * 2 + hf]
                            LD(u, u.ap, wsrc[t_][l, e, :, hf * 512:(hf + 1) * 512].rearrange("(k p) n -> p k n", p=128), q="pool")
                load_expert(0)
                for e in range(NE):
                    if e + 1 < NE:
                        load_expert(e + 1)
                    un = wun[(e % 2) * 6:(e % 2) * 6 + 6]
                    ye_ = yes[e % 2]
                    for (v, t0, ntl) in streams(b):
                        cap = 2 * ntl * 128 // NE
                        for i in range(t0, t0 + ntl):
                            V("dve", lambda g, i=i, cap=cap, e=e: g.tensor_scalar(out=Pm.ap[:, i, 0:cap], in0=iota[:, 0:cap], scalar1=RNK.ap[:, i, e:e + 1],
                                                                                  scalar2=MSK.ap[:, i, e:e + 1], op0=ALU.is_equal, op1=ALU.mult),
                              r=[cst, RNK, MSK], w=[Pm])
                    for c in range(8):
                        if c % 2 == 0:
                            p = psum()
                        o0 = (c % 2) * 256
                        for i in range(NTL):
                            V("pe", lambda g, p=p, c=c, i=i, o0=o0: g.matmul(p.ap[:, o0:o0 + capL], lhsT=H2.ap[:, NTC + i, c * 128:(c + 1) * 128],
                                                                             rhs=Pm.ap[:, NTC + i, 0:capL], start=(i == 0), stop=(i == NTL - 1)), r=[H2, Pm], w=[p])
                        if c % 2 == 1:
                            V("act", lambda g, p=p, c=c: g.copy(out=xeT.ap[:, c - 1:c + 1, 0:capL], in_=p.ap.rearrange("p (a n) -> p a n", a=2)[:, :, 0:capL]),
                              r=[p], w=[xeT])
                    p = psum()
                    for c in range(8):
                        for i in range(NTC):
                            V("pe", lambda g, p=p, c=c, i=i: g.matmul(p.ap[:, c * capC:(c + 1) * capC], lhsT=H2.ap[:, i, c * 128:(c + 1) * 128],
                                                                      rhs=Pm.ap[:, i, 0:capC], start=(i == 0), stop=(i == NTC - 1)), r=[H2, Pm], w=[p])
                    V("dve", lambda g, p=p: g.tensor_copy(out=xeT.ap[:, :, capL:NS], in_=p.ap[:, 0:8 * capC].rearrange("p (c n) -> p c n", c=8)), r=[p], w=[xeT])
                    for fc in range(8):
                        pg, pu = psum(), psum()
                        ug, uu = un[0 + fc // 4], un[2 + fc // 4]
                        fo = (fc % 4) * 128
                        for c in range(8):
                            V("pe", lambda g, pg=pg, c=c, ug=ug, fo=fo: g.matmul(pg.ap[:, 0:NS], lhsT=ug.ap[:, c, fo:fo + 128], rhs=xeT.ap[:, c, :],
                                                                                 start=(c == 0), stop=(c == 7)), r=[ug, xeT], w=[pg])
                        for c in range(8):
                            V("pe", lambda g, pu=pu, c=c, uu=uu, fo=fo: g.matmul(pu.ap[:, 0:NS], lhsT=uu.ap[:, c, fo:fo + 128], rhs=xeT.ap[:, c, :],
                                                                                 start=(c == 0), stop=(c == 7)), r=[uu, xeT], w=[pu])
                        V("act", lambda g, pg=pg: g.activation(out=sgt.ap, in_=pg.ap[:, 0:NS], func=AF.Silu), r=[pg], w=[sgt])
                        V("dve", lambda g, pu=pu, fc=fc: g.tensor_tensor(out=hidT.ap[:, fc, :], in0=sgt.ap, in1=pu.ap[:, 0:NS], op=ALU.mult), r=[sgt, pu], w=[hidT])
                    chunks = [(jc * JW, JW, jc) for jc in range(JL)] + [(capL, capC, JL)]
                    for (j0, jn, slot) in chunks:
                        for hf in range(2):
                            p = psum()
                            ud = un[4 + hf]
                            for fc in range(8):
                                V("pe", lambda g, p=p, fc=fc, j0=j0, jn=jn, ud=ud: g.matmul(p.ap[0:jn, :], lhsT=hidT.ap[:, fc, j0:j0 + jn], rhs=ud.ap[:, fc, :],
                                                                                         start=(fc == 0), stop=(fc == 7)), r=[hidT, ud], w=[p])
                            if hf == 0:
                                V("act", lambda g, p=p, jn=jn, slot=slot, ye_=ye_: g.copy(out=ye_.ap[0:jn, slot, 0:512], in_=p.ap[0:jn, :]), r=[p], w=[ye_])
                            else:
                                V("dve", lambda g, p=p, jn=jn, slot=slot, ye_=ye_: g.tensor_copy(out=ye_.ap[0:jn, slot, 512:1024], in_=p.ap[0:jn, :]), r=[p], w=[ye_])
                    for (j0, jn, slot) in chunks:
                        STO(ye_, YE[e, j0:j0 + jn, :], ye_.ap[0:jn, slot, :])
                em.barrier()
                chk()
                AR.off = AR_mid
                YA = AR.alloc([128, NE, JL + 1, D], BF16, "YA")
                for e in range(NE):
                    for jc in range(JL):
                        LD(YA, YA.ap[0:JW, e, jc, :], YE[e, jc * JW:(jc + 1) * JW, :])
                    LD(YA, YA.ap[0:capC, e, JL, :], YE[e, capL:NS, :])
                G2 = AR.alloc([128, D], F32, "G2")
                Pg = [AR.alloc([128, JL * JW], BF16, f"Pg{i}") for i in range(2)]
                GT = [AR.alloc([128, JL, 128], BF16, f"GT{i}") for i in range(2)]
                xts = [AR.alloc([128, D], F32, f"xs{i}") for i in range(2)]
                xo = [AR.alloc([128, D], F32, f"xq{i}") for i in range(2)]
                mt = AR.alloc([128, 512], F32, "mt2")
                it = 0
                ip = 0
                for (v, t0, ntl) in streams(b):
                    gate_tile(l, v, 1, G2)
                    is_lat = t0 >= NTC
                    cap = capL if is_lat else capC
                    chunks = [(jc * JW, JW, jc) for jc in range(JL)] if is_lat else [(0, capC, JL)]
                    for i in range(t0, t0 + ntl):
                        xt, xo_ = xts[it % 2], xo[it % 2]
                        it += 1
                        LD(xt, xt.ap, XS[b, i * 128:(i + 1) * 128, :])
                        po0, po1 = PS[0], PS[1]
                        for e in range(NE):
                            pg_, gt_ = Pg[ip % 2], GT[ip % 2]
                            ip += 1
                            V("dve", lambda g, i=i, e=e, cap=cap, pg_=pg_: g.tensor_scalar(out=pg_.ap[:, 0:cap], in0=iota[:, 0:cap], scalar1=RNK.ap[:, i, e:e + 1],
                                                                                        scalar2=GW.ap[:, i, e:e + 1], op0=ALU.is_equal, op1=ALU.mult),
                              r=[cst, RNK, GW], w=[pg_])
                            p = psum()
                            pb = bfv(p)
                            for ci, (j0, jn, slot) in enumerate(chunks):
                                V("pe", lambda g, pb=pb, ci=ci, j0=j0, jn=jn, pg_=pg_: g.transpose(out=pb[0:jn, ci * 128:(ci + 1) * 128], in_=pg_.ap[:, j0:j0 + jn], identity=ident_b),
                                  r=[pg_, cstb], w=[p])
                            jn = chunks[0][1]
                            ncn = len(chunks)
                            V("act", lambda g, pb=pb, jn=jn, ncn=ncn, gt_=gt_: g.copy(out=gt_.ap[0:jn, 0:ncn, :], in_=pb[0:jn, 0:ncn * 128].rearrange("p (c t) -> p c t", c=ncn)),
                              r=[p], w=[gt_])
                            for ci, (j0, jn, slot) in enumerate(chunks):
                                for hf, po in enumerate((po0, po1)):
                                    V("pe", lambda g, po=po, ci=ci, jn=jn, slot=slot, hf=hf, e=e, gt_=gt_, ncn=ncn: g.matmul(
                                        po.ap, lhsT=gt_.ap[0:jn, ci, :], rhs=YA.ap[0:jn, e, slot, hf * 512:(hf + 1) * 512],
                                        start=(e == 0 and ci == 0), stop=(e == NE - 1 and ci == ncn - 1)), r=[gt_, YA], w=[po])
                        for hf, po in enumerate((po0, po1)):
                            tt("dve", mt, mt.ap, po, po.ap, G2, G2.ap[:, hf * 512:(hf + 1) * 512], ALU.mult)
                            tt("dve", xo_, xo_.ap[:, hf * 512:(hf + 1) * 512], mt, mt.ap, xt, xt.ap[:, hf * 512:(hf + 1) * 512], ALU.add)
                        STO(xo_, XS[b, i * 128:(i + 1) * 128, :], xo_.ap)
                em.barrier()
                chk()
                AR.reset()

        fn = AR.alloc([128, D], F32, "fn")
        LD(fn, fn.ap, bc_row(0, I["final_norm"], D))
        xts = [AR.alloc([128, D], F32, f"xf{i}") for i in range(2)]
        junk = AR.alloc([128, D], F32, "junkf")
        stats = [AR.alloc([128, 2], F32, f"sf{i}") for i in range(2)]
        xo = [AR.alloc([128, D], F32, f"xfo{i}") for i in range(2)]
        it = 0
        for b in range(NB):
            for i in range(NTL):
                xt, stt_, xo_ = xts[it % 2], stats[it % 2], xo[it % 2]
                it += 1
                LD(xt, xt.ap, XS[b, C + i * 128:C + (i + 1) * 128, :])
                rms_scale(xt, stt_, junk, D, 1e-6)
                V("dve", lambda g, xt=xt, stt_=stt_, xo_=xo_: g.scalar_tensor_tensor(out=xo_.ap, in0=xt.ap, scalar=stt_.ap[:, 0:1], in1=fn.ap,
                                                                                     op0=ALU.mult, op1=ALU.mult), r=[xt, stt_, fn], w=[xo_])
                STO(xo_, OUT[b, i * 128:(i + 1) * 128, :], xo_.ap)
        em.barrier()
        em.finalize()
    return nc, em


def make_consts(T):
    cst = np.zeros((128, 768), np.float32)
    cst[:, 0:128] = np.eye(128, dtype=np.float32)
    cst[:, 128:256] = np.eye(128, dtype=np.float32)[::-1]
    tp = np.arange(128)[:, None]
    tt_ = np.arange(128)[None, :]
    cst[:, 256:384] = (tp < tt_).astype(np.float32)
    cst[:, 384:512] = 1.0
    cst[:, 512:768] = np.arange(256, dtype=np.float32)[None, :]
    t = np.arange(T)
    row = (t // 64).astype(np.float32)
    col = (t % 64).astype(np.float32)
    inv = (np.float32(10000.0) ** (-np.arange(0, 32, 2, dtype=np.float32) / np.float32(32))).astype(np.float32)
    ang_r = row[:, None] * inv[None, :]
    ang_c = col[:, None] * inv[None, :]
    rope = np.concatenate([np.cos(ang_r), np.cos(ang_c), np.sin(ang_r), np.sin(ang_c)], axis=1).astype(np.float32)
    return cst, rope


_CACHE = {}


def kernel(**inputs):
    x = np.asarray(inputs["x"], np.float32)
    B, T, _ = x.shape
    C = inputs["ctx"].shape[1]
    L = inputs["ada_w"].shape[0]
    NB = 2
    ncores = B // NB
    key = (T, C, L, NB)
    if key not in _CACHE:
        _CACHE[key] = build(T, C, L, NB)[0]
    nc = _CACHE[key]
    cst, rope = make_consts(T)
    shared = {}
    for k, v in inputs.items():
        if k in ("x", "ctx", "c"):
            continue
        a = np.ascontiguousarray(np.asarray(v, np.float32))
        if k in ("c_ctx", "final_norm"):
            a = a.reshape(1, -1)
        shared[k] = a
    shared["cst"] = cst
    shared["rope"] = rope
    in_maps = []
    for i in range(ncores):
        m = dict(shared)
        m["x"] = np.ascontiguousarray(x[i * NB:(i + 1) * NB])
        m["ctx"] = np.ascontiguousarray(np.asarray(inputs["ctx"], np.float32)[i * NB:(i + 1) * NB])
        m["c"] = np.ascontiguousarray(np.asarray(inputs["c"], np.float32)[i * NB:(i + 1) * NB])
        in_maps.append(m)
    res = run_bass_kernel_spmd(nc, in_maps, core_ids=list(range(ncores)))
    return np.concatenate([r["out"] for r in res.results], axis=0).astype(np.float32)
```

```python
import contextlib
import os
import numpy as np
import concourse.bass as bass
import concourse.mybir as mybir
from concourse.bass_utils import run_bass_kernel_spmd

F32 = mybir.dt.float32
BF16 = mybir.dt.bfloat16
ALU = mybir.AluOpType
AF = mybir.ActivationFunctionType
AX = mybir.AxisListType

D = 1024
KD = 8
INW = 7296
O_ATT, O_RW, O_CV, O_GT = 0, 768, 2688, 4224
NE = 16
DECAY_C = -0.6065306597126334
EMBED_WAIT = True


class Buf:
    __slots__ = ("name", "w", "r")

    def __init__(self, name=""):
        self.name = name
        self.w = None
        self.r = {}


class Tile:
    __slots__ = ("ap", "b")

    def __init__(self, ap, b):
        self.ap = ap
        self.b = b


class _Rec:
    def __init__(self):
        self.call = None

    def __getattr__(self, name):
        def f(*a, **k):
            self.call = (name, a, k)
            return self
        return f


class Em:
    ENG = ("pe", "act", "dve", "pool", "sp")
    NDMA = 8
    RESET_AT = 1 << 40

    def __init__(self, nc, stack):
        self.nc = nc
        self.sems = []
        self.val = []
        for e in self.ENG:
            self.sems.append(stack.enter_context(nc.semaphore("s_" + e)))
            self.val.append(0)
        self.cidx = {e: i for i, e in enumerate(self.ENG)}
        self.dma_pool = {}
        self.dma_rr = {}
        for q in ("sp", "pool"):
            ids = []
            for j in range(self.NDMA):
                self.sems.append(stack.enter_context(nc.semaphore(f"d_{q}{j}")))
                self.val.append(0)
                ids.append(len(self.sems) - 1)
            self.dma_pool[q] = ids
            self.dma_rr[q] = 0
        self.B1 = stack.enter_context(nc.semaphore("bar1"))
        self.B2 = stack.enter_context(nc.semaphore("bar2"))
        self.nreset = 0
        self.epoch = 0
        self.known = {e: {} for e in self.ENG}
        self.prog = {e: [] for e in self.ENG}
        self.ninst = 0
        self.muted = False

    def _waits(self, e, reads, writes, extra=()):
        need = {}

        def add(s, v):
            if need.get(s, 0) < v:
                need[s] = v
        for s, v in extra:
            add(s, v)
        ep = self.epoch
        for b in reads:
            if b.w is not None and b.w[2] == ep:
                add(b.w[0], b.w[1])
        for b in writes:
            if b.w is not None and b.w[2] == ep:
                add(b.w[0], b.w[1])
            for s, (v, e_) in b.r.items():
                if e_ == ep:
                    add(s, v)
        kn = self.known[e]
        own = self.cidx[e]
        out = []
        for s, v in need.items():
            if kn.get(s, 0) >= v:
                continue
            kn[s] = v
            if e == "pe" and s == own:
                continue
            out.append((s, v))
        return out

    def _mark(self, ev, reads, writes):
        s, v = ev
        ep = self.epoch
        for b in reads:
            cur = b.r.get(s)
            if cur is None or cur[1] != ep or cur[0] < v:
                b.r[s] = (v, ep)
        for b in writes:
            b.w = (s, v, ep)
            b.r = {}

    def op(self, e, fn, reads=(), writes=()):
        if self.muted:
            return None
        if max(self.val) >= self.RESET_AT:
            self.reset()
        w = self._waits(e, reads, writes)
        s = self.cidx[e]
        self.val[s] += 1
        ev = (s, self.val[s])
        rec = _Rec()
        fn(rec)
        name_, a_, k_ = rec.call
        self.prog[e].append((w, (lambda g, name_=name_, a_=a_, k_=k_: getattr(g, name_)(*a_, **k_)), s, 1))
        self._mark(ev, reads, writes)
        self.ninst += 1
        return ev

    def dma(self, q, out, in_, reads=(), writes=(), **kw):
        if self.muted:
            return None
        if max(self.val) >= self.RESET_AT:
            self.reset()
        ids = self.dma_pool[q]
        s = ids[self.dma_rr[q] % len(ids)]
        self.dma_rr[q] += 1
        w = self._waits(q, reads, writes, extra=[(s, self.val[s])] if self.val[s] else [])
        self.val[s] += 16
        ev = (s, self.val[s])
        self.prog[q].append((w, (lambda e, out=out, in_=in_, kw=kw: e.dma_start(out=out, in_=in_, **kw)), s, 16))
        self._mark(ev, reads, writes)
        self.ninst += 1
        return ev

    def barrier(self):
        for e in self.ENG:
            kn = self.known[e]
            w = []
            for s in range(len(self.sems)):
                v = self.val[s]
                if v > 0 and kn.get(s, 0) < v:
                    w.append((s, v))
                    kn[s] = v
            if w:
                self.prog[e].append((w, None, None, 0))

    def reset(self):
        self.barrier()
        self.nreset += 1
        k = self.nreset
        B1, B2, sems = self.B1, self.B2, self.sems
        for e in self.ENG:
            self.prog[e].append(([], (lambda g: g.nop().then_inc(B1, 1)), None, 0))
        self.prog["pool"].append(([(B1, 5 * k)], (lambda g: [g.sem_clear(h) for h in sems]), None, 0))
        self.prog["pool"].append(([], (lambda g: g.nop().then_inc(B2, 1)), None, 0))
        for e in self.ENG:
            self.prog[e].append(([(B2, k)], None, None, 0))
        self.val = [0] * len(self.val)
        self.known = {e: {} for e in self.ENG}
        self.epoch += 1

    def finalize(self):
        nc = self.nc
        sems = self.sems
        prog = self.prog

        def run(eng, lst):
            for (w, fn, s, inc) in lst:
                emb = None
                if fn is not None and s is not None and w and EMBED_WAIT:
                    emb, w = w[0], w[1:]
                for (ws, wv) in w:
                    eng.wait_ge(sems[ws] if isinstance(ws, int) else ws, wv)
                if fn is not None:
                    ins = fn(eng)
                    if emb is not None:
                        ins._wait_ge(sems[emb[0]], emb[1])
                    if s is not None:
                        ins.then_inc(sems[s], inc)
        with nc.Block() as block:
            @block.tensor
            def _(e):
                run(e, prog["pe"])

            @block.scalar
            def _(e):
                run(e, prog["act"])

            @block.vector
            def _(e):
                run(e, prog["dve"])

            @block.gpsimd
            def _(e):
                run(e, prog["pool"])

            @block.sync
            def _(e):
                run(e, prog["sp"])


class Arena:
    def __init__(self, nc, st, nbytes):
        self.t = st.enter_context(nc.sbuf_tensor("arena", [128, nbytes // 4], F32))
        self.cap = nbytes
        self.off = 0
        self.base = 0

    def alloc(self, shape, dt=F32, name=""):
        esz = 4 if dt == F32 else 2
        nel = int(np.prod(shape[1:]))
        n = (nel * esz + 63) // 64 * 64
        off = self.off
        self.off += n
        assert self.off <= self.cap, f"SBUF arena overflow {self.off} > {self.cap} ({name})"
        ap = self.t[0:shape[0], off // 4:(off + n) // 4]
        if dt != F32:
            ap = ap.bitcast(dt)
        ap = ap[:, 0:nel]
        if len(shape) > 2:
            names = [f"a{i}" for i in range(len(shape) - 1)]
            pat = "p (" + " ".join(names) + ") -> p " + " ".join(names)
            ap = ap.rearrange(pat, **{nm: int(s) for nm, s in zip(names[:-1], shape[1:-1])})
        return Tile(ap, Buf(name))

    def mark_base(self):
        self.base = self.off

    def reset(self):
        self.off = self.base


def dap(t, offset, dims):
    return bass.AP(t.tensor, int(offset), [[int(s), int(c)] for s, c in dims])


class _Stop(Exception):
    pass


def build(T=2048, C=256, L=4, NB=2, dump=(), upto=99):
    NT = T + C
    NTL, NTC, NTT = T // 128, C // 128, (T + C) // 128
    ZR = NT + 3
    capL, capC = 2 * T // NE, 2 * C // NE
    NS = capL + capC
    JL = (capL + 127) // 128
    JW = min(128, capL)
    QB = min(512, T)

    nc = bass.Bass("TRN2", target_bir_lowering=False)

    def din(name, shape):
        return nc.dram_tensor(name, list(shape), F32, kind="ExternalInput").ap()

    I = {}
    I["x"] = din("x", [NB, T, D])
    I["ctx"] = din("ctx", [NB, C, D])
    I["c"] = din("c", [NB, D])
    I["c_ctx"] = din("c_ctx", [1, D])
    for nm, sh in [("ada_w", [L, D, 6 * D]), ("ada_b", [L, 6 * D]), ("norm1", [L, D]), ("w_in", [L, D, INW]),
                   ("q_gain", [L, 64]), ("k_gain", [L, 64]), ("shift_mu", [L, 1920]), ("decay_w0", [L, 2, 512]),
                   ("decay_w2", [L, 2, 64, 512]), ("iclr_a0", [L, 2, 512]), ("iclr_a2", [L, 2, 64, 512]),
                   ("gate_g2", [L, 128, 512]), ("rwkv_kk", [L, 512]), ("rwkv_ka", [L, 512]), ("rwkv_rk", [L, 512]),
                   ("rwkv_gn_w", [L, 512]), ("rwkv_gn_b", [L, 512]), ("conv_w", [L, 3, 512]),
                   ("w_br_att", [L, 512, D]), ("w_br_rwkv", [L, 512, D]), ("w_br_conv", [L, 512, D]),
                   ("w_out", [L, D, D]), ("norm2", [L, D]), ("w_router", [L, D, NE]),
                   ("exp_gate", [L, NE, D, D]), ("exp_up", [L, NE, D, D]), ("exp_down", [L, NE, D, D]),
                   ("final_norm", [1, D]), ("rope", [T, 64]), ("cst", [128, 768])]:
        I[nm] = din(nm, sh)
    OUT = nc.dram_tensor("out", [NB, T, D], F32, kind="ExternalOutput").ap()

    def dscr(name, shape, dt=F32):
        if name in dump:
            return nc.dram_tensor(name, list(shape), dt, kind="ExternalOutput").ap()
        return nc.dram_tensor(name, list(shape), dt).ap()

    XS = dscr("XS", [NB, NT, D])
    ZIN = dscr("ZIN", [NB, ZR, INW])
    MODV = dscr("MODV", [L, 3, 6 * D])
    SI = dscr("SI", [NB * 16, NT, 6, 64])
    YO = dscr("YO", [4, NB * 16, NT, 16])
    RWAUX = dscr("RWAUX", [NB, NT, 1024])
    OBR = dscr("OBR", [NB, NT, 3, 512])
    YE = dscr("YE", [NE, NS, D], BF16)

    def zrow(n):
        return n + 1 if n < C else n + 2

    with contextlib.ExitStack() as st:
        em = Em(nc, st)
        stage_ctr = [0]

        dbg_done = set()

        def dbg(name, tile_, shape, dt=F32):
            if 'DBG' not in dump or name in dbg_done:
                return
            dbg_done.add(name)
            o = nc.dram_tensor('dbg_' + name, list(shape), dt, kind='ExternalOutput').ap()
            em.dma('sp', o, tile_.ap, reads=[tile_.b])

        def stop(label):
            if os.environ.get('K_STOP') == label:
                em.muted = True

        def chk():
            stage_ctr[0] += 1
            if stage_ctr[0] > upto:
                em.muted = True
        AR = Arena(nc, st, 184 * 1024)
        PS = []
        for i in range(8):
            t = st.enter_context(nc.psum_tensor(f"ps{i}", [128, 512], F32))
            PS.append(Tile(t[:, :], Buf(f"ps{i}")))
        ps_rr = [0]

        def psum():
            p = PS[4 + ps_rr[0] % 4]
            ps_rr[0] += 1
            return p

        def bfv(p):
            return p.ap.bitcast(BF16)

        def V(e, fn, r=(), w=()):
            return em.op(e, fn, reads=[t.b for t in r], writes=[t.b for t in w])

        def LD(out_t, out_ap, in_ap, q="sp"):
            return em.dma(q, out_ap, in_ap, writes=[out_t.b])

        def STO(in_t, out_ap, in_ap, q="pool"):
            return em.dma(q, out_ap, in_ap, reads=[in_t.b])

        def tt(e, out_t, out, in0_t, in0, in1_t, in1, op):
            return V(e, lambda g: g.tensor_tensor(out=out, in0=in0, in1=in1, op=op), r=[in0_t, in1_t], w=[out_t])

        def bc_row(dram_ap_1d_offset, tensor_ap, n, parts=128):
            return dap(tensor_ap, dram_ap_1d_offset, [[0, parts], [1, n]])

        cst = AR.alloc([128, 768], F32, "cst")
        LD(cst, cst.ap, I["cst"])
        ident = cst.ap[:, 0:128]
        Jm = cst.ap[:, 128:256]
        iota = cst.ap[:, 512:768]
        cstb = AR.alloc([128, 512], BF16, "cstb")
        V("dve", lambda g: g.tensor_copy(out=cstb.ap, in_=cst.ap[:, 0:512]), r=[cst], w=[cstb])
        ident_b = cstb.ap[:, 0:128]
        U_b = cstb.ap[:, 256:384]
        ones_b = cstb.ap[:, 384:512]
        rope = AR.alloc([128, NTL, 64], F32, "rope")
        LD(rope, rope.ap, I["rope"].rearrange("(i p) f -> p i f", p=128))
        AR.mark_base()

        for b in range(NB):
            em.dma("sp", XS[b, 0:C, :], I["ctx"][b])
            em.dma("sp", XS[b, C:NT, :], I["x"][b])
        zt = AR.alloc([1, INW], F32, "zt")
        V("dve", lambda g: g.memset(zt.ap, 0.0), w=[zt])
        for b in range(NB):
            for r in (0, C + 1, ZR - 1):
                STO(zt, ZIN[b, r:r + 1, :], zt.ap)
        c2 = AR.alloc([3, D], F32, "c2")
        for b in range(NB):
            LD(c2, c2.ap[b:b + 1, :], I["c"][b:b + 1, :])
        LD(c2, c2.ap[2:3, :], I["c_ctx"])
        V("act", lambda g: g.activation(out=c2.ap, in_=c2.ap, func=AF.Silu), r=[c2], w=[c2])
        cT = AR.alloc([128, 8, 3], BF16, "cT")
        p = psum()
        for k in range(8):
            V("pe", lambda g, k=k: g.transpose(out=p.ap[:, k * 3:(k + 1) * 3], in_=c2.ap[0:3, k * 128:(k + 1) * 128],
                                               identity=ident[0:3, 0:3]), r=[c2, cst], w=[p])
        V("dve", lambda g: g.tensor_copy(out=cT.ap, in_=p.ap[:, 0:24].rearrange("p (k v) -> p k v", v=3)), r=[p], w=[cT])
        wb = [AR.alloc([128, 8, 512], BF16, f"adaw{i}") for i in range(2)]
        bb = [AR.alloc([3, 512], F32, f"adab{i}") for i in range(2)]
        mo = [AR.alloc([3, 512], F32, f"mo{i}") for i in range(2)]
        it = 0
        for l in range(L):
            for j in range(12):
                w_, b_, m_ = wb[it % 2], bb[it % 2], mo[it % 2]
                it += 1
                LD(w_, w_.ap, I["ada_w"][l, :, j * 512:(j + 1) * 512].rearrange("(k p) n -> p k n", p=128), q="pool")
                LD(b_, b_.ap, bc_row(l * 6 * D + j * 512, I["ada_b"], 512, parts=3))
                p = psum()
                for k in range(8):
                    V("pe", lambda g, k=k, p=p, w_=w_: g.matmul(p.ap[0:3, :], lhsT=cT.ap[:, k, :], rhs=w_.ap[:, k, :],
                                                             start=(k == 0), stop=(k == 7)), r=[cT, w_], w=[p])
                tt("dve", m_, m_.ap, p, p.ap[0:3, :], b_, b_.ap, ALU.add)
                STO(m_, MODV[l, :, j * 512:(j + 1) * 512], m_.ap)
        em.barrier()
        chk()
        AR.reset()

        def mod_tiles(l, v, which, A_t, B_t, tmp_t):
            base = (l * 3 + v) * 6 * D + which * 3 * D
            LD(B_t, B_t.ap, bc_row(base, MODV, D))
            LD(tmp_t, tmp_t.ap, bc_row(base + D, MODV, D))
            nrm = I["norm1"] if which == 0 else I["norm2"]
            LD(A_t, A_t.ap, bc_row(l * D, nrm, D))
            V("dve", lambda g: g.scalar_tensor_tensor(out=A_t.ap, in0=tmp_t.ap, scalar=1.0, in1=A_t.ap, op0=ALU.add,
                                                      op1=ALU.mult), r=[tmp_t, A_t], w=[A_t])

        def gate_tile(l, v, which, G_t):
            base = (l * 3 + v) * 6 * D + which * 3 * D + 2 * D
            LD(G_t, G_t.ap, bc_row(base, MODV, D))

        def rms_scale(x_t, stat_t, junk_t, width, eps, ncols=1):
            V("dve", lambda g: g.memset(stat_t.ap[:, 0:1], 0.0), w=[stat_t])
            V("act", lambda g: g.activation(out=junk_t.ap, in_=x_t.ap, func=AF.Square, accum_out=stat_t.ap[:, 0:1]),
              r=[x_t, stat_t], w=[junk_t, stat_t])
            V("dve", lambda g: g.tensor_scalar(out=stat_t.ap[:, 0:1], in0=stat_t.ap[:, 0:1], scalar1=1.0 / width,
                                               scalar2=eps, op0=ALU.mult, op1=ALU.add), r=[stat_t], w=[stat_t])
            V("act", lambda g: g.activation(out=stat_t.ap[:, 0:1], in_=stat_t.ap[:, 0:1], func=AF.Sqrt), r=[stat_t], w=[stat_t])
            V("dve", lambda g: g.reciprocal(out=stat_t.ap[:, 0:1], in_=stat_t.ap[:, 0:1]), r=[stat_t], w=[stat_t])

        def streams(b):
            return [(2, 0, NTC), (b, NTC, NTL)]

        for l in range(L):
            HT = AR.alloc([128, 8, NB * NT], BF16, "HT")
            A_t = AR.alloc([128, D], F32, "A1")
            B_t = AR.alloc([128, D], F32, "B1")
            tmpm = AR.alloc([128, D], F32, "tmpm")
            xts = [AR.alloc([128, D], F32, f"xt{i}") for i in range(2)]
            junk = AR.alloc([128, D], F32, "junk")
            hbs = [AR.alloc([128, D], BF16, f"hb{i}") for i in range(2)]
            stats = [AR.alloc([128, 2], F32, f"st{i}") for i in range(2)]
            it = 0
            for b in range(NB):
                for (v, t0, ntl) in streams(b):
                    mod_tiles(l, v, 0, A_t, B_t, tmpm)
                    dbg('A1', A_t, [128, D])
                    dbg('B1', B_t, [128, D])
                    for i in range(t0, t0 + ntl):
                        xt, hb, stt_ = xts[it % 2], hbs[it % 2], stats[it % 2]
                        it += 1
                        LD(xt, xt.ap, XS[b, i * 128:(i + 1) * 128, :])
                        dbg('xt0', xt, [128, D])
                        rms_scale(xt, stt_, junk, D, 1e-6)
                        dbg('st0', stt_, [128, 2])
                        V("dve", lambda g, xt=xt, stt_=stt_: g.scalar_tensor_tensor(out=xt.ap, in0=xt.ap, scalar=stt_.ap[:, 0:1],
                                                                                    in1=A_t.ap, op0=ALU.mult, op1=ALU.mult),
                          r=[xt, stt_, A_t], w=[xt])
                        dbg('xt1', xt, [128, D])
                        tt("dve", hb, hb.ap, xt, xt.ap, B_t, B_t.ap, ALU.add)
                        dbg('hb', hb, [128, D], BF16)
                        p = psum()
                        pb = bfv(p)
                        for k in range(8):
                            V("pe", lambda g, k=k, hb=hb, pb=pb: g.transpose(out=pb[:, k * 128:(k + 1) * 128],
                                                                             in_=hb.ap[:, k * 128:(k + 1) * 128], identity=ident_b),
                              r=[hb, cstb], w=[p])
                        col = b * NT + i * 128
                        V("act", lambda g, pb=pb, col=col: g.copy(out=HT.ap[:, :, col:col + 128],
                                                                  in_=pb.rearrange("p (k t) -> p k t", k=8)), r=[p], w=[HT])
            WBLK = 512
            wbl = [AR.alloc([128, 8, WBLK], BF16, f"wbl{i}") for i in range(2)]
            zst = [AR.alloc([128, WBLK], F32, f"zst{i}") for i in range(3)]
            it = 0
            wblocks = [(c0, min(WBLK, INW - c0)) for c0 in range(0, INW, WBLK)]
            for j, (c0, cw_) in enumerate(wblocks):
                w_ = wbl[j % 2]
                LD(w_, w_.ap[:, :, 0:cw_], I["w_in"][l, :, c0:c0 + cw_].rearrange("(k p) n -> p k n", p=128), q="pool")
                for b in range(NB):
                    for i in range(NTT):
                        p = psum()
                        col = b * NT + i * 128
                        for k in range(8):
                            V("pe", lambda g, k=k, p=p, w_=w_, col=col, cw_=cw_: g.matmul(p.ap[:, 0:cw_], lhsT=HT.ap[:, k, col:col + 128],
                                                                                        rhs=w_.ap[:, k, 0:cw_], start=(k == 0), stop=(k == 7)),
                              r=[HT, w_], w=[p])
                        z_ = zst[it % 3]
                        eng = "act" if it % 2 == 0 else "dve"
                        it += 1
                        if eng == "act":
                            V("act", lambda g, z_=z_, p=p, cw_=cw_: g.copy(out=z_.ap[:, 0:cw_], in_=p.ap[:, 0:cw_]), r=[p], w=[z_])
                        else:
                            V("dve", lambda g, z_=z_, p=p, cw_=cw_: g.tensor_copy(out=z_.ap[:, 0:cw_], in_=p.ap[:, 0:cw_]), r=[p], w=[z_])
                        r0 = zrow(i * 128)
                        STO(z_, ZIN[b, r0:r0 + 128, c0:c0 + cw_], z_.ap[:, 0:cw_], q="sp")
            em.barrier()
            chk()
            AR.reset()

            gq = AR.alloc([128, 10, 64], F32, "gq")
            LD(gq, gq.ap[:, 0:8, :], dap(I["q_gain"], l * 64, [[0, 128], [0, 8], [1, 64]]))
            LD(gq, gq.ap[:, 8:10, :], dap(I["k_gain"], l * 64, [[0, 128], [0, 2], [1, 64]]))
            qT = AR.alloc([64, 8, NT], BF16, "qT")
            kT = AR.alloc([64, 2, NT], BF16, "kT")
            vA = AR.alloc([128, NTT, 2, 65], BF16, "vA")
            zts = [AR.alloc([128, 768], F32, f"zt{i}") for i in range(2)]
            sq = AR.alloc([128, 640], F32, "sq")
            ss = AR.alloc([128, 10], F32, "ss")
            qk = AR.alloc([128, 10, 64], F32, "qk")
            r1 = AR.alloc([128, 10, 2, 16], F32, "r1")
            r2 = AR.alloc([128, 10, 2, 16], F32, "r2")
            qkb = AR.alloc([128, 10, 64], BF16, "qkb")
            pTs = [AR.alloc([128, QB], BF16, f"pT{i}") for i in range(2)]
            oatt = AR.alloc([128, QB // 128, 512], F32, "oatt")
            rec = AR.alloc([128, 4, 1], F32, "rec")
            for b in range(NB):
                V("dve", lambda g: g.memset(vA.ap[:, :, :, 64:65], 1.0), w=[vA])
                for i in range(NTT):
                    z = zts[i % 2]
                    r0 = zrow(i * 128)
                    LD(z, z.ap, ZIN[b, r0:r0 + 128, 0:768])
                    V("act", lambda g, z=z: g.activation(out=sq.ap, in_=z.ap[:, 0:640], func=AF.Square), r=[z], w=[sq])
                    V("dve", lambda g: g.tensor_reduce(out=ss.ap, in_=sq.ap.rearrange("p (h d) -> p h d", d=64), axis=AX.X,
                                                       op=ALU.add), r=[sq], w=[ss])
                    V("dve", lambda g: g.tensor_scalar(out=ss.ap, in0=ss.ap, scalar1=1.0 / 64, scalar2=1e-6, op0=ALU.mult,
                                                       op1=ALU.add), r=[ss], w=[ss])
                    V("act", lambda g: g.activation(out=ss.ap, in_=ss.ap, func=AF.Sqrt), r=[ss], w=[ss])
                    V("dve", lambda g: g.reciprocal(out=ss.ap, in_=ss.ap), r=[ss], w=[ss])
                    V("dve", lambda g, z=z: g.tensor_tensor(out=qk.ap, in0=z.ap[:, 0:640].rearrange("p (h d) -> p h d", d=64),
                                                            in1=ss.ap.unsqueeze(2).broadcast_to([128, 10, 64]), op=ALU.mult),
                      r=[z, ss], w=[qk])
                    tt("dve", qk, qk.ap, qk, qk.ap, gq, gq.ap, ALU.mult)
                    if i >= NTC:
                        il = i - NTC
                        q5 = qk.ap.rearrange("p h (a x f) -> p h a x f", a=2, x=2)
                        o5 = qkb.ap.rearrange("p h (a x f) -> p h a x f", a=2, x=2)
                        x1, x2 = q5[:, :, :, 0, :], q5[:, :, :, 1, :]
                        cos = rope.ap[:, il, 0:32].rearrange("p (a f) -> p a f", a=2).unsqueeze(1).broadcast_to([128, 10, 2, 16])
                        sin = rope.ap[:, il, 32:64].rearrange("p (a f) -> p a f", a=2).unsqueeze(1).broadcast_to([128, 10, 2, 16])
                        tt("dve", r1, r1.ap, qk, x1, rope, cos, ALU.mult)
                        tt("dve", r2, r2.ap, qk, x2, rope, sin, ALU.mult)
                        tt("dve", qkb, o5[:, :, :, 0, :], r1, r1.ap, r2, r2.ap, ALU.subtract)
                        tt("dve", r1, r1.ap, qk, x1, rope, sin, ALU.mult)
                        tt("dve", r2, r2.ap, qk, x2, rope, cos, ALU.mult)
                        tt("dve", qkb, o5[:, :, :, 1, :], r1, r1.ap, r2, r2.ap, ALU.add)
                    else:
                        V("dve", lambda g: g.tensor_copy(out=qkb.ap, in_=qk.ap), r=[qk], w=[qkb])
                    V("act", lambda g, z=z, i=i: g.copy(out=vA.ap[:, i, :, 0:64], in_=z.ap[:, 640:768].rearrange("p (h d) -> p h d", d=64)),
                      r=[z], w=[vA])
                    p1, p2 = psum(), psum()
                    pb1, pb2 = bfv(p1), bfv(p2)
                    for h in range(10):
                        dst = pb1[0:64, h * 128:(h + 1) * 128] if h < 8 else pb2[0:64, (h - 8) * 128:(h - 7) * 128]
                        V("pe", lambda g, h=h, dst=dst: g.transpose(out=dst, in_=qkb.ap[:, h, :], identity=ident_b),
                          r=[qkb, cstb], w=[p1 if h < 8 else p2])
                    V("act", lambda g, pb1=pb1, i=i: g.copy(out=qT.ap[:, :, i * 128:(i + 1) * 128],
                                                            in_=pb1[0:64, :].rearrange("p (h t) -> p h t", h=8)), r=[p1], w=[qT])
                    V("dve", lambda g, pb2=pb2, i=i: g.tensor_copy(out=kT.ap[:, :, i * 128:(i + 1) * 128],
                                                                   in_=pb2[0:64, 0:256].rearrange("p (h t) -> p h t", h=2)), r=[p2], w=[kT])
                blocks = [(0, C, NTC)] + [(C + qb * QB, QB, NTT) for qb in range(T // QB)]
                itp = 0
                for (q0, qn, nkt) in blocks:
                    nsub = qn // 128
                    for h in range(8):
                        kvh = h // 4
                        for s in range(nkt):
                            p = psum()
                            V("pe", lambda g, p=p, s=s, h=h, kvh=kvh, q0=q0, qn=qn: g.matmul(
                                p.ap[:, 0:qn], lhsT=kT.ap[:, kvh, s * 128:(s + 1) * 128], rhs=qT.ap[:, h, q0:q0 + qn],
                                start=True, stop=True), r=[kT, qT], w=[p])
                            pT = pTs[itp % 2]
                            itp += 1
                            V("act", lambda g, p=p, pT=pT, qn=qn: g.activation(out=pT.ap[:, 0:qn], in_=p.ap[:, 0:qn], func=AF.Exp,
                                                                               scale=0.125), r=[p], w=[pT])
                            for j in range(nsub):
                                V("pe", lambda g, j=j, s=s, pT=pT, kvh=kvh, nkt=nkt: g.matmul(
                                    PS[j].ap[:, 0:65], lhsT=pT.ap[:, j * 128:(j + 1) * 128], rhs=vA.ap[:, s, kvh, :],
                                    start=(s == 0), stop=(s == nkt - 1)), r=[pT, vA], w=[PS[j]])
                        for j in range(nsub):
                            V("dve", lambda g, j=j: g.reciprocal(out=rec.ap[:, j, :], in_=PS[j].ap[:, 64:65]), r=[PS[j]], w=[rec])
                            V("dve", lambda g, j=j, h=h: g.tensor_scalar(out=oatt.ap[:, j, h * 64:(h + 1) * 64], in0=PS[j].ap[:, 0:64], scalar1=rec.ap[:, j, :],
                                                                         scalar2=None, op0=ALU.mult), r=[PS[j], rec], w=[oatt])
                    for j in range(nsub):
                        n0 = q0 + j * 128
                        STO(oatt, OBR[b, n0:n0 + 128, 0, :], oatt.ap[:, j, :])
            em.barrier()
            chk()
            AR.reset()

            mu = AR.alloc([128, 1920], F32, "mu")
            LD(mu, mu.ap, bc_row(l * 1920, I["shift_mu"], 1920))
            w0 = AR.alloc([128, 2, 512], F32, "w0")
            LD(w0, w0.ap, bc_row(l * 1024, I["decay_w0"], 1024).rearrange("p (d c) -> p d c", d=2))
            a0 = AR.alloc([128, 2, 512], F32, "a0")
            LD(a0, a0.ap, bc_row(l * 1024, I["iclr_a0"], 1024).rearrange("p (d c) -> p d c", d=2))
            pkk = AR.alloc([128, 512], F32, "pkk")
            LD(pkk, pkk.ap, bc_row(l * 512, I["rwkv_kk"], 512))
            pka = AR.alloc([128, 512], F32, "pka")
            LD(pka, pka.ap, bc_row(l * 512, I["rwkv_ka"], 512))
            prk = AR.alloc([128, 512], F32, "prk")
            LD(prk, prk.ap, bc_row(l * 512, I["rwkv_rk"], 512))
            w2 = AR.alloc([64, 2, 512], BF16, "w2")
            LD(w2, w2.ap, I["decay_w2"][l].rearrange("d r c -> r d c"), q="pool")
            a2 = AR.alloc([64, 2, 512], BF16, "a2")
            LD(a2, a2.ap, I["iclr_a2"][l].rearrange("d r c -> r d c"), q="pool")
            g2w = AR.alloc([128, 512], BF16, "g2w")
            LD(g2w, g2w.ap, I["gate_g2"][l], q="pool")
            curs = [AR.alloc([128, 1920], F32, f"cur{i}") for i in range(2)]
            prvs = [AR.alloc([128, 1920], F32, f"prv{i}") for i in range(2)]
            nxts = [AR.alloc([128, 1920], F32, f"nxt{i}") for i in range(2)]
            lr_2 = [AR.alloc([128, 384], BF16, f"lr{i}") for i in range(2)]
            lrT_2 = [AR.alloc([128, 5, 128], BF16, f"lrT{i}") for i in range(2)]
            tA_2 = [AR.alloc([128, 512], F32, f"tA{i}") for i in range(2)]
            tB_2 = [AR.alloc([128, 512], F32, f"tB{i}") for i in range(2)]
            kkt_2 = [AR.alloc([128, 512], F32, f"kkt{i}") for i in range(2)]
            s8_2 = [AR.alloc([128, 8], F32, f"s8{i}") for i in range(2)]
            ad_2 = [[AR.alloc([128, 512], F32, f"ad{d}{i}") for d in range(2)] for i in range(2)]
            sts_2 = [[AR.alloc([128, 8, 6, 64], F32, f"sis{d}{i}") for d in range(2)] for i in range(2)]
            stf_2 = [AR.alloc([128, 8 * 6 * 64], F32, f"stf{i}") for i in range(2)]
            aux_2 = [AR.alloc([128, 1024], F32, f"aux{i}") for i in range(2)]

            def v3(ap2d):
                return ap2d.rearrange("p (h d) -> p h d", d=64)

            it = 0
            for b in range(NB):
                for (v, t0, ntl) in streams(b):
                    seg0, seg1 = t0 * 128, (t0 + ntl) * 128
                    for i in range(t0, t0 + ntl):
                        cur, prv, nxt = curs[it % 2], prvs[it % 2], nxts[it % 2]
                        par = it % 2
                        lr, lrT, tA, tB, kkt, s8, stf, aux = [x[par] for x in (lr_2, lrT_2, tA_2, tB_2, kkt_2, s8_2, stf_2, aux_2)]
                        ad, sts = ad_2[par], sts_2[par]
                        it += 1
                        r0 = zrow(i * 128)
                        LD(cur, cur.ap, ZIN[b, r0:r0 + 128, O_RW:O_RW + 1920])
                        LD(prv, prv.ap, ZIN[b, r0 - 1:r0 + 127, O_RW:O_RW + 1920])
                        LD(nxt, nxt.ap, ZIN[b, r0 + 1:r0 + 129, O_RW:O_RW + 1920])
                        tt("dve", prv, prv.ap, prv, prv.ap, nxt, nxt.ap, ALU.add)
                        V("dve", lambda g, prv=prv, cur=cur: g.scalar_tensor_tensor(out=prv.ap, in0=prv.ap, scalar=0.5, in1=cur.ap,
                                                                                    op0=ALU.mult, op1=ALU.subtract), r=[prv, cur], w=[prv])
                        tt("dve", prv, prv.ap, prv, prv.ap, mu, mu.ap, ALU.mult)
                        tt("dve", cur, cur.ap, prv, prv.ap, cur, cur.ap, ALU.add)
                        seg = cur
                        R_, K_, V_ = seg.ap[:, 0:512], seg.ap[:, 512:1024], seg.ap[:, 1024:1536]
                        V("act", lambda g, seg=seg: g.activation(out=lr.ap[:, 0:128], in_=seg.ap[:, 1536:1664], func=AF.Tanh), r=[seg], w=[lr])
                        V("dve", lambda g, seg=seg: g.tensor_copy(out=lr.ap[:, 128:256], in_=seg.ap[:, 1664:1792]), r=[seg], w=[lr])
                        V("act", lambda g, seg=seg: g.activation(out=lr.ap[:, 256:384], in_=seg.ap[:, 1792:1920], func=AF.Sigmoid), r=[seg], w=[lr])
                        p = psum()
                        pb = bfv(p)
                        for k in range(4):
                            V("pe", lambda g, k=k, pb=pb: g.transpose(out=pb[0:64, k * 128:(k + 1) * 128], in_=lr.ap[:, k * 64:(k + 1) * 64],
                                                                      identity=ident_b), r=[lr, cstb], w=[p])
                        V("pe", lambda g, pb=pb: g.transpose(out=pb[:, 512:640], in_=lr.ap[:, 256:384], identity=ident_b), r=[lr, cstb], w=[p])
                        V("dve", lambda g, pb=pb: g.tensor_copy(out=lrT.ap[0:64, 0:4, :], in_=pb[0:64, 0:512].rearrange("p (k t) -> p k t", k=4)), r=[p], w=[lrT])
                        V("dve", lambda g, pb=pb: g.tensor_copy(out=lrT.ap[:, 4, :], in_=pb[:, 512:640]), r=[p], w=[lrT])
                        tt("dve", kkt, kkt.ap, seg, K_, pkk, pkk.ap, ALU.mult)
                        V("act", lambda g: g.activation(out=tA.ap, in_=kkt.ap, func=AF.Square), r=[kkt], w=[tA])
                        V("dve", lambda g: g.tensor_reduce(out=s8.ap, in_=v3(tA.ap), axis=AX.X, op=ALU.add), r=[tA], w=[s8])
                        V("dve", lambda g: g.tensor_scalar(out=s8.ap, in0=s8.ap, scalar1=1e-12, scalar2=None, op0=ALU.add), r=[s8], w=[s8])
                        V("act", lambda g: g.activation(out=s8.ap, in_=s8.ap, func=AF.Sqrt), r=[s8], w=[s8])
                        V("dve", lambda g: g.reciprocal(out=s8.ap, in_=s8.ap), r=[s8], w=[s8])
                        V("dve", lambda g: g.tensor_tensor(out=v3(kkt.ap), in0=v3(kkt.ap), in1=s8.ap.unsqueeze(2).broadcast_to([128, 8, 64]),
                                                           op=ALU.mult), r=[kkt, s8], w=[kkt])
                        for d in range(2):
                            sd = sts[d]
                            lo, hi = d * 64, (d + 1) * 64
                            p = psum()
                            V("pe", lambda g, p=p, d=d: g.matmul(p.ap, lhsT=lrT.ap[0:64, d, :], rhs=w2.ap[:, d, :], start=True, stop=True),
                              r=[lrT, w2], w=[p])
                            tt("dve", tA, tA.ap, p, p.ap, w0, w0.ap[:, d, :], ALU.add)
                            V("act", lambda g: g.activation(out=tA.ap, in_=tA.ap, func=AF.Sigmoid), r=[tA], w=[tA])
                            V("act", lambda g, sd=sd: g.activation(out=sd.ap[:, :, 1, :], in_=v3(tA.ap), func=AF.Exp, scale=DECAY_C), r=[tA], w=[sd])
                            p = psum()
                            V("pe", lambda g, p=p, d=d: g.matmul(p.ap, lhsT=lrT.ap[0:64, 2 + d, :], rhs=a2.ap[:, d, :], start=True, stop=True),
                              r=[lrT, a2], w=[p])
                            a_ = ad[d]
                            tt("dve", a_, a_.ap, p, p.ap, a0, a0.ap[:, d, :], ALU.add)
                            V("act", lambda g, a_=a_: g.activation(out=a_.ap, in_=a_.ap, func=AF.Sigmoid), r=[a_], w=[a_])
                            V("dve", lambda g, a_=a_: g.scalar_tensor_tensor(out=tB.ap, in0=a_.ap, scalar=-1.0, in1=pka.ap, op0=ALU.add, op1=ALU.mult),
                              r=[a_, pka], w=[tB])
                            V("dve", lambda g, sd=sd, K_=K_: g.scalar_tensor_tensor(out=sd.ap[:, :, 3, :], in0=v3(tB.ap), scalar=1.0, in1=v3(K_),
                                                                                    op0=ALU.add, op1=ALU.mult), r=[tB, seg], w=[sd])
                            V("dve", lambda g, sd=sd, a_=a_: g.tensor_tensor(out=sd.ap[:, :, 2, :], in0=v3(kkt.ap), in1=v3(a_.ap), op=ALU.mult),
                              r=[kkt, a_], w=[sd])
                            V("dve", lambda g, sd=sd: g.tensor_scalar(out=sd.ap[:, :, 0, :], in0=v3(kkt.ap), scalar1=-1.0, scalar2=None, op0=ALU.mult),
                              r=[kkt], w=[sd])
                            V("act", lambda g, sd=sd, R_=R_: g.copy(out=sd.ap[:, :, 4, :], in_=v3(R_)), r=[seg], w=[sd])
                            V("act", lambda g, sd=sd, V_=V_: g.copy(out=sd.ap[:, :, 5, :], in_=v3(V_)), r=[seg], w=[sd])
                        p = psum()
                        V("pe", lambda g, p=p: g.matmul(p.ap, lhsT=lrT.ap[:, 4, :], rhs=g2w.ap, start=True, stop=True), r=[lrT, g2w], w=[p])
                        V("act", lambda g, p=p: g.copy(out=aux.ap[:, 512:1024], in_=p.ap), r=[p], w=[aux])
                        tt("dve", tA, tA.ap, seg, R_, seg, K_, ALU.mult)
                        tt("dve", tA, tA.ap, tA, tA.ap, prk, prk.ap, ALU.mult)
                        V("dve", lambda g: g.tensor_reduce(out=s8.ap, in_=v3(tA.ap), axis=AX.X, op=ALU.add), r=[tA], w=[s8])
                        V("dve", lambda g, V_=V_: g.tensor_tensor(out=v3(aux.ap[:, 0:512]), in0=v3(V_), in1=s8.ap.unsqueeze(2).broadcast_to([128, 8, 64]),
                                                                  op=ALU.mult), r=[seg, s8], w=[aux])
                        STO(aux, RWAUX[b, i * 128:(i + 1) * 128, :], aux.ap)
                        ch0 = (b * 2 + 0) * 8
                        STO(sts[0], dap(SI, (ch0 * NT + i * 128) * 384, [[384, 128], [NT * 384, 8], [1, 384]]),
                            sts[0].ap.rearrange("p h s d -> p h (s d)"))
                        s1f = sts[1].ap.rearrange("p h s d -> p (h s d)")
                        for cblk in range(6):
                            p = psum()
                            V("pe", lambda g, p=p, cblk=cblk, s1f=s1f: g.matmul(p.ap, lhsT=Jm, rhs=s1f[:, cblk * 512:(cblk + 1) * 512], start=True, stop=True),
                              r=[sts[1], cst], w=[p])
                            if cblk % 2 == 0:
                                V("act", lambda g, p=p, cblk=cblk: g.copy(out=stf.ap[:, cblk * 512:(cblk + 1) * 512], in_=p.ap), r=[p], w=[stf])
                            else:
                                V("dve", lambda g, p=p, cblk=cblk: g.tensor_copy(out=stf.ap[:, cblk * 512:(cblk + 1) * 512], in_=p.ap), r=[p], w=[stf])
                        ch1 = (b * 2 + 1) * 8
                        s0 = seg0 + seg1 - 128 - i * 128
                        STO(stf, dap(SI, (ch1 * NT + s0) * 384, [[384, 128], [NT * 384, 8], [1, 384]]),
                            stf.ap.rearrange("p (h x) -> p h x", h=8))
            em.barrier()
            chk()
            AR.reset()

            CH = 16
            S_ = AR.alloc([128, 16, 64], F32, "S")
            Ta = AR.alloc([128, 16, 64], F32, "Ta")
            Tr = AR.alloc([128, 16, 64], F32, "Tr")
            U_ = AR.alloc([128, 2, 16, 64], F32, "U")
            sic = [AR.alloc([128, CH, 5, 64], F32, f"sic{i}") for i in range(2)]
            vbc = [AR.alloc([128, CH, 2, 16], F32, f"vbc{i}") for i in range(2)]
            yoc = [AR.alloc([128, CH, 16], F32, f"yoc{i}") for i in range(2)]
            V("dve", lambda g: g.memset(S_.ap, 0.0), w=[S_])
            NCH = NB * 16
            nchunks = NT // CH

            def scan_loads(c):
                si, vb = sic[c % 2], vbc[c % 2]
                s0 = c * CH
                for vq in range(4):
                    em.dma("sp", si.ap[vq * 32:vq * 32 + NCH], dap(SI, s0 * 384, [[NT * 384, NCH], [384, CH], [1, 320]]).rearrange("c s (a k) -> c s a k", a=5),
                           writes=[si.b])
                    em.dma("sp", vb.ap[vq * 32:vq * 32 + NCH, :, 1, :], dap(SI, s0 * 384 + 320 + vq * 16, [[NT * 384, NCH], [384, CH], [1, 16]]),
                           writes=[vb.b])

            def emit_ta(c, s):
                si = sic[c % 2]
                tt("dve", Ta, Ta.ap, S_, S_.ap, si, si.ap[:, s, 0, :].unsqueeze(1).broadcast_to([128, 16, 64]), ALU.mult)
            scan_loads(0)
            emit_ta(0, 0)
            for c in range(nchunks):
                if c + 1 < nchunks:
                    scan_loads(c + 1)
                si, vb, yo = sic[c % 2], vbc[c % 2], yoc[c % 2]
                s0 = c * CH
                for s in range(CH):
                    W_b = si.ap[:, s, 1, :].unsqueeze(1).broadcast_to([128, 16, 64])
                    R_b = si.ap[:, s, 4, :].unsqueeze(1).broadcast_to([128, 16, 64])
                    BK = si.ap[:, s, 2:4, :].unsqueeze(2).broadcast_to([128, 2, 16, 64])
                    SAV = vb.ap[:, s, :, :].unsqueeze(3).broadcast_to([128, 2, 16, 64])
                    V("dve", lambda g, vb=vb, s=s: g.tensor_reduce(out=vb.ap[:, s, 0, :], in_=Ta.ap, axis=AX.X, op=ALU.add), r=[Ta], w=[vb])
                    tt("dve", S_, S_.ap, S_, S_.ap, si, W_b, ALU.mult)
                    V("dve", lambda g, SAV=SAV, BK=BK: g.tensor_tensor(out=U_.ap, in0=SAV, in1=BK, op=ALU.mult), r=[vb, si], w=[U_])
                    tt("dve", S_, S_.ap, S_, S_.ap, U_, U_.ap[:, 0], ALU.add)
                    tt("dve", S_, S_.ap, S_, S_.ap, U_, U_.ap[:, 1], ALU.add)
                    tt("dve", Tr, Tr.ap, S_, S_.ap, si, R_b, ALU.mult)
                    if s + 1 < CH:
                        emit_ta(c, s + 1)
                    elif c + 1 < nchunks:
                        emit_ta(c + 1, 0)
                    V("dve", lambda g, yo=yo, s=s: g.tensor_reduce(out=yo.ap[:, s, :], in_=Tr.ap, axis=AX.X, op=ALU.add), r=[Tr], w=[yo])
                for vq in range(4):
                    em.dma("pool", dap(YO, (vq * NCH * NT + s0) * 16, [[NT * 16, NCH], [16, CH], [1, 16]]), yo.ap[vq * 32:vq * 32 + NCH],
                           reads=[yo.b])
            em.barrier()
            chk()
            AR.reset()

            gnw = AR.alloc([128, 512], F32, "gnw")
            LD(gnw, gnw.ap, bc_row(l * 512, I["rwkv_gn_w"], 512))
            gnb = AR.alloc([128, 512], F32, "gnb")
            LD(gnb, gnb.ap, bc_row(l * 512, I["rwkv_gn_b"], 512))
            cw = AR.alloc([128, 3, 512], F32, "cw")
            LD(cw, cw.ap, bc_row(l * 1536, I["conv_w"], 1536).rearrange("p (j c) -> p j c", j=3))
            y0s = [AR.alloc([128, 512], F32, f"y0{i}") for i in range(2)]
            y1s = [AR.alloc([128, 512], F32, f"y1{i}") for i in range(2)]
            axs = [AR.alloc([128, 1024], F32, f"ax{i}") for i in range(2)]
            yy_2 = [AR.alloc([128, 512], F32, f"yy{i}") for i in range(2)]
            ysq_2 = [AR.alloc([128, 512], F32, f"ysq{i}") for i in range(2)]
            m8_2 = [AR.alloc([128, 8], F32, f"m8{i}") for i in range(2)]
            orw = [AR.alloc([128, 512], F32, f"orw{i}") for i in range(2)]
            ccs = [AR.alloc([128, 1536], F32, f"cc{i}") for i in range(2)]
            cps = [AR.alloc([128, 1024], F32, f"cp{i}") for i in range(2)]
            cns = [AR.alloc([128, 1024], F32, f"cn{i}") for i in range(2)]
            ocv = [AR.alloc([128, 512], F32, f"ocv{i}") for i in range(2)]
            it = 0
            for b in range(NB):
                for (v, t0, ntl) in streams(b):
                    seg0, seg1 = t0 * 128, (t0 + ntl) * 128
                    for i in range(t0, t0 + ntl):
                        y0, y1, ax, o_ = y0s[it % 2], y1s[it % 2], axs[it % 2], orw[it % 2]
                        cc, cp, cn, oc = ccs[it % 2], cps[it % 2], cns[it % 2], ocv[it % 2]
                        yy, ysq, m8 = yy_2[it % 2], ysq_2[it % 2], m8_2[it % 2]
                        it += 1
                        ch0, ch1 = (b * 2) * 8, (b * 2 + 1) * 8
                        s1 = seg0 + seg1 - 128 - i * 128
                        for vq in range(4):
                            em.dma("sp", y0.ap.rearrange("p (h q f) -> p h q f", h=8, q=4)[:, :, vq, :],
                                   dap(YO, ((vq * NCH + ch0) * NT + i * 128) * 16, [[16, 128], [NT * 16, 8], [1, 16]]), writes=[y0.b])
                            em.dma("sp", y1.ap.rearrange("p (h q f) -> p h q f", h=8, q=4)[:, :, vq, :],
                                   dap(YO, ((vq * NCH + ch1) * NT + s1) * 16, [[16, 128], [NT * 16, 8], [1, 16]]), writes=[y1.b])
                        LD(ax, ax.ap, RWAUX[b, i * 128:(i + 1) * 128, :])
                        p = psum()
                        V("pe", lambda g, p=p, y1=y1: g.matmul(p.ap, lhsT=Jm, rhs=y1.ap, start=True, stop=True), r=[y1, cst], w=[p])
                        tt("dve", yy, yy.ap, y0, y0.ap, p, p.ap, ALU.add)
                        V("dve", lambda g: g.tensor_reduce(out=m8.ap, in_=v3(yy.ap), axis=AX.X, op=ALU.add), r=[yy], w=[m8])
                        V("dve", lambda g: g.tensor_scalar(out=m8.ap, in0=m8.ap, scalar1=-1.0 / 64, scalar2=None, op0=ALU.mult), r=[m8], w=[m8])
                        V("dve", lambda g: g.tensor_tensor(out=v3(yy.ap), in0=v3(yy.ap), in1=m8.ap.unsqueeze(2).broadcast_to([128, 8, 64]), op=ALU.add),
                          r=[yy, m8], w=[yy])
                        V("act", lambda g: g.activation(out=ysq.ap, in_=yy.ap, func=AF.Square), r=[yy], w=[ysq])
                        V("dve", lambda g: g.tensor_reduce(out=m8.ap, in_=v3(ysq.ap), axis=AX.X, op=ALU.add), r=[ysq], w=[m8])
                        V("dve", lambda g: g.tensor_scalar(out=m8.ap, in0=m8.ap, scalar1=1.0 / 64, scalar2=64e-5, op0=ALU.mult, op1=ALU.add), r=[m8], w=[m8])
                        V("act", lambda g: g.activation(out=m8.ap, in_=m8.ap, func=AF.Sqrt), r=[m8], w=[m8])
                        V("dve", lambda g: g.reciprocal(out=m8.ap, in_=m8.ap), r=[m8], w=[m8])
                        V("dve", lambda g: g.tensor_tensor(out=v3(yy.ap), in0=v3(yy.ap), in1=m8.ap.unsqueeze(2).broadcast_to([128, 8, 64]), op=ALU.mult),
                          r=[yy, m8], w=[yy])
                        tt("dve", yy, yy.ap, yy, yy.ap, gnw, gnw.ap, ALU.mult)
                        tt("dve", yy, yy.ap, yy, yy.ap, gnb, gnb.ap, ALU.add)
                        tt("dve", yy, yy.ap, yy, yy.ap, ax, ax.ap[:, 0:512], ALU.add)
                        tt("dve", o_, o_.ap, yy, yy.ap, ax, ax.ap[:, 512:1024], ALU.mult)
                        STO(o_, OBR[b, i * 128:(i + 1) * 128, 1, :], o_.ap)
                        r0 = zrow(i * 128)
                        LD(cc, cc.ap, ZIN[b, r0:r0 + 128, O_CV:O_CV + 1536])
                        LD(cp, cp.ap, ZIN[b, r0 - 1:r0 + 127, O_CV + 512:O_CV + 1536])
                        LD(cn, cn.ap, ZIN[b, r0 + 1:r0 + 129, O_CV + 512:O_CV + 1536])
                        tt("dve", cc, cc.ap[:, 512:1024], cc, cc.ap[:, 512:1024], cc, cc.ap[:, 1024:1536], ALU.mult)
                        tt("dve", cp, cp.ap[:, 0:512], cp, cp.ap[:, 0:512], cp, cp.ap[:, 512:1024], ALU.mult)
                        tt("dve", cn, cn.ap[:, 0:512], cn, cn.ap[:, 0:512], cn, cn.ap[:, 512:1024], ALU.mult)
                        tt("dve", cc, cc.ap[:, 512:1024], cc, cc.ap[:, 512:1024], cw, cw.ap[:, 1, :], ALU.mult)
                        tt("dve", cp, cp.ap[:, 0:512], cp, cp.ap[:, 0:512], cw, cw.ap[:, 0, :], ALU.mult)
                        tt("dve", cn, cn.ap[:, 0:512], cn, cn.ap[:, 0:512], cw, cw.ap[:, 2, :], ALU.mult)
                        tt("dve", cc, cc.ap[:, 512:1024], cc, cc.ap[:, 512:1024], cp, cp.ap[:, 0:512], ALU.add)
                        tt("dve", cc, cc.ap[:, 512:1024], cc, cc.ap[:, 512:1024], cn, cn.ap[:, 0:512], ALU.add)
                        tt("dve", oc, oc.ap, cc, cc.ap[:, 512:1024], cc, cc.ap[:, 0:512], ALU.mult)
                        STO(oc, OBR[b, i * 128:(i + 1) * 128, 2, :], oc.ap)
            em.barrier()
            chk()
            AR.reset()

            wbr = [AR.alloc([128, 4, D], BF16, f"wbr{i}") for i in range(3)]
            for i_, nm in enumerate(("w_br_att", "w_br_rwkv", "w_br_conv")):
                LD(wbr[i_], wbr[i_].ap, I[nm][l].rearrange("(k p) n -> p k n", p=128), q="pool")
            wo = AR.alloc([128, 8, D], BF16, "wo")
            LD(wo, wo.ap, I["w_out"][l].rearrange("(k p) n -> p k n", p=128), q="pool")
            G1 = AR.alloc([128, D], F32, "G1")
            obs = [AR.alloc([128, 1536], F32, f"ob{i}") for i in range(2)]
            gts = [AR.alloc([128, 3072], F32, f"gt{i}") for i in range(2)]
            xts = [AR.alloc([128, D], F32, f"xm{i}") for i in range(2)]
            obb_2 = [AR.alloc([128, 1536], BF16, f"obb{i}") for i in range(2)]
            oT_2 = [AR.alloc([128, 12, 128], BF16, f"oT{i}") for i in range(2)]
            mm_2 = [AR.alloc([128, D], F32, f"mm{i}") for i in range(2)]
            mt_2 = [AR.alloc([128, 512], F32, f"mt{i}") for i in range(2)]
            mb_2 = [AR.alloc([128, D], BF16, f"mb{i}") for i in range(2)]
            mT_2 = [AR.alloc([128, 8, 128], BF16, f"mT{i}") for i in range(2)]
            xo = [AR.alloc([128, D], F32, f"xo{i}") for i in range(2)]
            it = 0
            for b in range(NB):
                for (v, t0, ntl) in streams(b):
                    gate_tile(l, v, 0, G1)
                    for i in range(t0, t0 + ntl):
                        ob, gt, xt, xo_ = obs[it % 2], gts[it % 2], xts[it % 2], xo[it % 2]
                        obb, oT, mm, mt, mb, mT = [x[it % 2] for x in (obb_2, oT_2, mm_2, mt_2, mb_2, mT_2)]
                        it += 1
                        r0 = zrow(i * 128)
                        LD(ob, ob.ap, OBR[b, i * 128:(i + 1) * 128, :, :].rearrange("p a c -> p (a c)"))
                        LD(gt, gt.ap, ZIN[b, r0:r0 + 128, O_GT:O_GT + 3072])
                        LD(xt, xt.ap, XS[b, i * 128:(i + 1) * 128, :])
                        V("dve", lambda g, ob=ob: g.tensor_copy(out=obb.ap, in_=ob.ap), r=[ob], w=[obb])
                        V("act", lambda g, gt=gt: g.activation(out=gt.ap, in_=gt.ap, func=AF.Sigmoid), r=[gt], w=[gt])
                        p1, p2 = psum(), psum()
                        pb1, pb2 = bfv(p1), bfv(p2)
                        for k in range(12):
                            dst = pb1[:, k * 128:(k + 1) * 128] if k < 8 else pb2[:, (k - 8) * 128:(k - 7) * 128]
                            V("pe", lambda g, k=k, dst=dst: g.transpose(out=dst, in_=obb.ap[:, k * 128:(k + 1) * 128], identity=ident_b),
                              r=[obb, cstb], w=[p1 if k < 8 else p2])
                        V("act", lambda g, pb1=pb1: g.copy(out=oT.ap[:, 0:8, :], in_=pb1.rearrange("p (k t) -> p k t", k=8)), r=[p1], w=[oT])
                        V("dve", lambda g, pb2=pb2: g.tensor_copy(out=oT.ap[:, 8:12, :], in_=pb2[:, 0:512].rearrange("p (k t) -> p k t", k=4)), r=[p2], w=[oT])
                        for br in range(3):
                            for hf in range(2):
                                p = psum()
                                for k in range(4):
                                    V("pe", lambda g, p=p, k=k, br=br, hf=hf: g.matmul(p.ap, lhsT=oT.ap[:, br * 4 + k, :],
                                                                                       rhs=wbr[br].ap[:, k, hf * 512:(hf + 1) * 512],
                                                                                       start=(k == 0), stop=(k == 3)), r=[oT, wbr[br]], w=[p])
                                gsl = gt.ap[:, br * 1024 + hf * 512:br * 1024 + (hf + 1) * 512]
                                if br == 0:
                                    tt("dve", mm, mm.ap[:, hf * 512:(hf + 1) * 512], p, p.ap, gt, gsl, ALU.mult)
                                else:
                                    tt("dve", mt, mt.ap, p, p.ap, gt, gsl, ALU.mult)
                                    tt("dve", mm, mm.ap[:, hf * 512:(hf + 1) * 512], mm, mm.ap[:, hf * 512:(hf + 1) * 512], mt, mt.ap, ALU.add)
                        V("act", lambda g: g.copy(out=mb.ap, in_=mm.ap), r=[mm], w=[mb])
                        p = psum()
                        pb = bfv(p)
                        for k in range(8):
                            V("pe", lambda g, k=k, pb=pb: g.transpose(out=pb[:, k * 128:(k + 1) * 128], in_=mb.ap[:, k * 128:(k + 1) * 128], identity=ident_b),
                              r=[mb, cstb], w=[p])
                        V("act", lambda g, pb=pb: g.copy(out=mT.ap, in_=pb.rearrange("p (k t) -> p k t", k=8)), r=[p], w=[mT])
                        for hf in range(2):
                            p = psum()
                            for k in range(8):
                                V("pe", lambda g, p=p, k=k, hf=hf: g.matmul(p.ap, lhsT=mT.ap[:, k, :], rhs=wo.ap[:, k, hf * 512:(hf + 1) * 512],
                                                                            start=(k == 0), stop=(k == 7)), r=[mT, wo], w=[p])
                            tt("dve", mt, mt.ap, p, p.ap, G1, G1.ap[:, hf * 512:(hf + 1) * 512], ALU.mult)
                            tt("dve", xo_, xo_.ap[:, hf * 512:(hf + 1) * 512], mt, mt.ap, xt, xt.ap[:, hf * 512:(hf + 1) * 512], ALU.add)
                        STO(xo_, XS[b, i * 128:(i + 1) * 128, :], xo_.ap)
            em.barrier()
            chk()
            AR.reset()

            for b in range(NB):
                H2 = AR.alloc([128, NTT, D], BF16, "H2")
                AFF = AR.alloc([128, NTT, NE], F32, "AFF")
                MSK = AR.alloc([128, NTT, NE], F32, "MSK")
                RNK = AR.alloc([128, NTT, NE], F32, "RNK")
                GW = AR.alloc([128, NTT, NE], F32, "GW")
                wr = AR.alloc([128, 8, NE], F32, "wr")
                LD(wr, wr.ap, I["w_router"][l].rearrange("(k p) e -> p k e", p=128))
                AR_mid = AR.off
                A_t = AR.alloc([128, D], F32, "A2")
                B_t = AR.alloc([128, D], F32, "B2")
                tmpm = AR.alloc([128, D], F32, "tmpm2")
                xts = [AR.alloc([128, D], F32, f"xn{i}") for i in range(2)]
                junk = AR.alloc([128, D], F32, "junk2")
                h2f = AR.alloc([128, D], F32, "h2f")
                h2T = AR.alloc([128, 8, 128], F32, "h2T")
                stats = [AR.alloc([128, 2], F32, f"sn{i}") for i in range(2)]
                lg = AR.alloc([128, NE], F32, "lg")
                mx = AR.alloc([128, 2], F32, "mx")
                affT = AR.alloc([NE, max(T, C)], F32, "affT")
                mkT = AR.alloc([NE, max(T, C)], F32, "mkT")
                bj = AR.alloc([NE, max(T, C)], F32, "bj")
                bis = AR.alloc([NE, 8], F32, "bis")
                mskb = AR.alloc([128, NTT, NE], BF16, "mskb")
                tot = AR.alloc([128, NTT, NE], F32, "tot")
                pre = AR.alloc([128, NTT, NE], F32, "pre")
                it = 0
                for (v, t0, ntl) in streams(b):
                    mod_tiles(l, v, 1, A_t, B_t, tmpm)
                    for i in range(t0, t0 + ntl):
                        xt, stt_ = xts[it % 2], stats[it % 2]
                        it += 1
                        LD(xt, xt.ap, XS[b, i * 128:(i + 1) * 128, :])
                        rms_scale(xt, stt_, junk, D, 1e-6)
                        V("dve", lambda g, xt=xt, stt_=stt_: g.scalar_tensor_tensor(out=xt.ap, in0=xt.ap, scalar=stt_.ap[:, 0:1], in1=A_t.ap,
                                                                                    op0=ALU.mult, op1=ALU.mult), r=[xt, stt_, A_t], w=[xt])
                        tt("dve", h2f, h2f.ap, xt, xt.ap, B_t, B_t.ap, ALU.add)
                        V("act", lambda g, i=i: g.copy(out=H2.ap[:, i, :], in_=h2f.ap), r=[h2f], w=[H2])
                        p1, p2 = psum(), psum()
                        for k in range(8):
                            pp = p1 if k < 4 else p2
                            V("pe", lambda g, k=k, pp=pp: g.transpose(out=pp.ap[:, (k % 4) * 128:(k % 4 + 1) * 128], in_=h2f.ap[:, k * 128:(k + 1) * 128],
                                                                      identity=ident), r=[h2f, cst], w=[pp])
                        V("act", lambda g, p1=p1: g.copy(out=h2T.ap[:, 0:4, :], in_=p1.ap.rearrange("p (k t) -> p k t", k=4)), r=[p1], w=[h2T])
                        V("dve", lambda g, p2=p2: g.tensor_copy(out=h2T.ap[:, 4:8, :], in_=p2.ap.rearrange("p (k t) -> p k t", k=4)), r=[p2], w=[h2T])
                        p = psum()
                        for k in range(8):
                            V("pe", lambda g, k=k, p=p: g.matmul(p.ap[:, 0:NE], lhsT=h2T.ap[:, k, :], rhs=wr.ap[:, k, :], start=(k == 0), stop=(k == 7)),
                              r=[h2T, wr], w=[p])
                        V("dve", lambda g, p=p: g.tensor_reduce(out=mx.ap[:, 0:1], in_=p.ap[:, 0:NE], axis=AX.X, op=ALU.max), r=[p], w=[mx])
                        V("dve", lambda g: g.tensor_scalar(out=mx.ap[:, 0:1], in0=mx.ap[:, 0:1], scalar1=-1.0, scalar2=None, op0=ALU.mult), r=[mx], w=[mx])
                        V("dve", lambda g: g.memset(mx.ap[:, 1:2], 0.0), w=[mx])
                        V("act", lambda g, p=p: g.activation(out=lg.ap, in_=p.ap[:, 0:NE], func=AF.Exp, bias=mx.ap[:, 0:1], scale=1.0,
                                                             accum_out=mx.ap[:, 1:2]), r=[p, mx], w=[lg, mx])
                        V("dve", lambda g: g.reciprocal(out=mx.ap[:, 1:2], in_=mx.ap[:, 1:2]), r=[mx], w=[mx])
                        V("dve", lambda g, i=i: g.tensor_scalar(out=AFF.ap[:, i, :], in0=lg.ap, scalar1=mx.ap[:, 1:2], scalar2=None, op0=ALU.mult),
                          r=[lg, mx], w=[AFF])
                stop('8a')
                for (v, t0, ntl) in streams(b):
                    ntok = ntl * 128
                    cap = 2 * ntok // NE
                    for i in range(ntl):
                        if i % 4 == 0:
                            p = psum()
                        V("pe", lambda g, p=p, i=i, t0=t0: g.transpose(out=p.ap[0:NE, (i % 4) * 128:(i % 4 + 1) * 128], in_=AFF.ap[:, t0 + i, :], identity=ident),
                          r=[AFF, cst], w=[p])
                        if i % 4 == 3 or i == ntl - 1:
                            n_ = (i % 4 + 1) * 128
                            c0 = (i // 4) * 512
                            V("dve", lambda g, p=p, n_=n_, c0=c0: g.tensor_copy(out=affT.ap[:, c0:c0 + n_], in_=p.ap[0:NE, 0:n_]), r=[p], w=[affT])
                    stop('8b1_%d' % t0)
                    V("dve", lambda g: g.memset(bis.ap[:, 0:1], 0.0), w=[bis])
                    V("dve", lambda g: g.memset(bis.ap[:, 1:2], 1.0), r=[bis], w=[bis])
                    for _ in range(32):
                        V("dve", lambda g: g.tensor_scalar(out=bis.ap[:, 2:3], in0=bis.ap[:, 0:1], scalar1=bis.ap[:, 1:2], scalar2=0.5, op0=ALU.add, op1=ALU.mult),
                          r=[bis], w=[bis])
                        V("dve", lambda g: g.memset(bis.ap[:, 3:4], 0.0), r=[bis], w=[bis])
                        V("dve", lambda g, ntok=ntok: g.tensor_scalar(out=bj.ap[:, 0:ntok], in0=affT.ap[:, 0:ntok], scalar1=bis.ap[:, 2:3], scalar2=0.0,
                                                                      op0=ALU.is_ge, op1=ALU.add, accum_out=bis.ap[:, 3:4]), r=[affT, bis], w=[bj, bis])
                        V("dve", lambda g, cap=cap: g.tensor_scalar(out=bis.ap[:, 4:5], in0=bis.ap[:, 3:4], scalar1=cap - 0.5, scalar2=None, op0=ALU.is_ge),
                          r=[bis], w=[bis])
                        V("dve", lambda g: g.tensor_tensor(out=bis.ap[:, 5:6], in0=bis.ap[:, 2:3], in1=bis.ap[:, 0:1], op=ALU.subtract), r=[bis], w=[bis])
                        V("dve", lambda g: g.scalar_tensor_tensor(out=bis.ap[:, 0:1], in0=bis.ap[:, 5:6], scalar=bis.ap[:, 4:5], in1=bis.ap[:, 0:1],
                                                                  op0=ALU.mult, op1=ALU.add), r=[bis], w=[bis])
                        V("dve", lambda g: g.tensor_tensor(out=bis.ap[:, 5:6], in0=bis.ap[:, 1:2], in1=bis.ap[:, 2:3], op=ALU.subtract), r=[bis], w=[bis])
                        V("dve", lambda g: g.scalar_tensor_tensor(out=bis.ap[:, 1:2], in0=bis.ap[:, 5:6], scalar=bis.ap[:, 4:5], in1=bis.ap[:, 2:3],
                                                                  op0=ALU.mult, op1=ALU.add), r=[bis], w=[bis])
                    stop('8b2_%d' % t0)
                    V("dve", lambda g, ntok=ntok: g.tensor_scalar(out=mkT.ap[:, 0:ntok], in0=affT.ap[:, 0:ntok], scalar1=bis.ap[:, 0:1], scalar2=None, op0=ALU.is_ge),
                      r=[affT, bis], w=[mkT])
                    p = psum()
                    for i in range(ntl):
                        V("pe", lambda g, p=p, i=i: g.transpose(out=p.ap[:, i * NE:(i + 1) * NE], in_=mkT.ap[0:NE, i * 128:(i + 1) * 128], identity=ident[0:NE, 0:NE]),
                          r=[mkT, cst], w=[p])
                    V("dve", lambda g, p=p, t0=t0, ntl=ntl: g.tensor_copy(out=MSK.ap[:, t0:t0 + ntl, :], in_=p.ap[:, 0:ntl * NE].rearrange("p (i e) -> p i e", e=NE)),
                      r=[p], w=[MSK])
                    V("act", lambda g, t0=t0, ntl=ntl: g.copy(out=mskb.ap[:, t0:t0 + ntl, :], in_=MSK.ap[:, t0:t0 + ntl, :]), r=[MSK], w=[mskb])
                    stop('8b3_%d' % t0)
                    pt_, pu_ = psum(), psum()
                    for i in range(ntl):
                        V("pe", lambda g, i=i, t0=t0, pt_=pt_: g.matmul(pt_.ap[:, i * NE:(i + 1) * NE], lhsT=ones_b, rhs=mskb.ap[:, t0 + i, :], start=True, stop=True),
                          r=[mskb, cstb], w=[pt_])
                        V("pe", lambda g, i=i, t0=t0, pu_=pu_: g.matmul(pu_.ap[:, i * NE:(i + 1) * NE], lhsT=U_b, rhs=mskb.ap[:, t0 + i, :], start=True, stop=True),
                          r=[mskb, cstb], w=[pu_])
                    V("dve", lambda g, pt_=pt_, t0=t0, ntl=ntl: g.tensor_copy(out=tot.ap[:, t0:t0 + ntl, :], in_=pt_.ap[:, 0:ntl * NE].rearrange("p (i e) -> p i e", e=NE)),
                      r=[pt_], w=[tot])
                    V("dve", lambda g, t0=t0: g.memset(pre.ap[:, t0, :], 0.0), w=[pre])
                    for i in range(1, ntl):
                        V("dve", lambda g, i=i, t0=t0: g.tensor_tensor(out=pre.ap[:, t0 + i, :], in0=pre.ap[:, t0 + i - 1, :], in1=tot.ap[:, t0 + i - 1, :], op=ALU.add),
                          r=[pre, tot], w=[pre])
                    V("dve", lambda g, pu_=pu_, t0=t0, ntl=ntl: g.tensor_tensor(out=RNK.ap[:, t0:t0 + ntl, :], in0=pu_.ap[:, 0:ntl * NE].rearrange("p (i e) -> p i e", e=NE),
                                                                                in1=pre.ap[:, t0:t0 + ntl, :], op=ALU.add), r=[pu_, pre], w=[RNK])
                    stop('8b4_%d' % t0)
                stop('8b5')
                tt("dve", GW, GW.ap, AFF, AFF.ap, MSK, MSK.ap, ALU.mult)
                em.barrier()
                chk()
                AR.off = AR_mid
                NU = 12
                wun = [AR.alloc([128, 8, 512], BF16, f"wu{i}") for i in range(NU)]
                Pm = AR.alloc([128, NTT, JL * JW], BF16, "Pm")
                xeT = AR.alloc([128, 8, NS], BF16, "xeT")
                sgt = AR.alloc([128, NS], F32, "sgt")
                hidT = AR.alloc([128, 8, NS], BF16, "hidT")
                yes = [AR.alloc([128, JL + 1, D], BF16, f"yes{i}") for i in range(2)]
                wsrc = (I["exp_gate"], I["exp_up"], I["exp_down"])

                def load_expert(e):
                    for t_ in range(3):
                        for hf in range(2):
                            u = wun[(e % 2) * 6 + t_ * 2 + hf]
                            LD(u, u.ap, wsrc[t_][l, e, :, hf * 512:(hf + 1) * 512].rearrange("(k p) n -> p k n", p=128), q="pool")
                load_expert(0)
                for e in range(NE):
                    if e + 1 < NE:
                        load_expert(e + 1)
                    un = wun[(e % 2) * 6:(e % 2) * 6 + 6]
                    ye_ = yes[e % 2]
                    for (v, t0, ntl) in streams(b):
                        cap = 2 * ntl * 128 // NE
                        for i in range(t0, t0 + ntl):
                            V("dve", lambda g, i=i, cap=cap, e=e: g.tensor_scalar(out=Pm.ap[:, i, 0:cap], in0=iota[:, 0:cap], scalar1=RNK.ap[:, i, e:e + 1],
                                                                                  scalar2=MSK.ap[:, i, e:e + 1], op0=ALU.is_equal, op1=ALU.mult),
                              r=[cst, RNK, MSK], w=[Pm])
                    for c in range(8):
                        if c % 2 == 0:
                            p = psum()
                        o0 = (c % 2) * 256
                        for i in range(NTL):
                            V("pe", lambda g, p=p, c=c, i=i, o0=o0: g.matmul(p.ap[:, o0:o0 + capL], lhsT=H2.ap[:, NTC + i, c * 128:(c + 1) * 128],
                                                                             rhs=Pm.ap[:, NTC + i, 0:capL], start=(i == 0), stop=(i == NTL - 1)), r=[H2, Pm], w=[p])
                        if c % 2 == 1:
                            V("act", lambda g, p=p, c=c: g.copy(out=xeT.ap[:, c - 1:c + 1, 0:capL], in_=p.ap.rearrange("p (a n) -> p a n", a=2)[:, :, 0:capL]),
                              r=[p], w=[xeT])
                    p = psum()
                    for c in range(8):
                        for i in range(NTC):
                            V("pe", lambda g, p=p, c=c, i=i: g.matmul(p.ap[:, c * capC:(c + 1) * capC], lhsT=H2.ap[:, i, c * 128:(c + 1) * 128],
                                                                      rhs=Pm.ap[:, i, 0:capC], start=(i == 0), stop=(i == NTC - 1)), r=[H2, Pm], w=[p])
                    V("dve", lambda g, p=p: g.tensor_copy(out=xeT.ap[:, :, capL:NS], in_=p.ap[:, 0:8 * capC].rearrange("p (c n) -> p c n", c=8)), r=[p], w=[xeT])
                    for fc in range(8):
                        pg, pu = psum(), psum()
                        ug, uu = un[0 + fc // 4], un[2 + fc // 4]
                        fo = (fc % 4) * 128
                        for c in range(8):
                            V("pe", lambda g, pg=pg, c=c, ug=ug, fo=fo: g.matmul(pg.ap[:, 0:NS], lhsT=ug.ap[:, c, fo:fo + 128], rhs=xeT.ap[:, c, :],
                                                                                 start=(c == 0), stop=(c == 7)), r=[ug, xeT], w=[pg])
                        for c in range(8):
                            V("pe", lambda g, pu=pu, c=c, uu=uu, fo=fo: g.matmul(pu.ap[:, 0:NS], lhsT=uu.ap[:, c, fo:fo + 128], rhs=xeT.ap[:, c, :],
                                                                                 start=(c == 0), stop=(c == 7)), r=[uu, xeT], w=[pu])
                        V("act", lambda g, pg=pg: g.activation(out=sgt.ap, in_=pg.ap[:, 0:NS], func=AF.Silu), r=[pg], w=[sgt])
                        V("dve", lambda g, pu=pu, fc=fc: g.tensor_tensor(out=hidT.ap[:, fc, :], in0=sgt.ap, in1=pu.ap[:, 0:NS], op=ALU.mult), r=[sgt, pu], w=[hidT])
                    chunks = [(jc * JW, JW, jc) for jc in range(JL)] + [(capL, capC, JL)]
                    for (j0, jn, slot) in chunks:
                        for hf in range(2):
                            p = psum()
                            ud = un[4 + hf]
                            for fc in range(8):
                                V("pe", lambda g, p=p, fc=fc, j0=j0, jn=jn, ud=ud: g.matmul(p.ap[0:jn, :], lhsT=hidT.ap[:, fc, j0:j0 + jn], rhs=ud.ap[:, fc, :],
                                                                                         start=(fc == 0), stop=(fc == 7)), r=[hidT, ud], w=[p])
                            if hf == 0:
                                V("act", lambda g, p=p, jn=jn, slot=slot, ye_=ye_: g.copy(out=ye_.ap[0:jn, slot, 0:512], in_=p.ap[0:jn, :]), r=[p], w=[ye_])
                            else:
                                V("dve", lambda g, p=p, jn=jn, slot=slot, ye_=ye_: g.tensor_copy(out=ye_.ap[0:jn, slot, 512:1024], in_=p.ap[0:jn, :]), r=[p], w=[ye_])
                    for (j0, jn, slot) in chunks:
                        STO(ye_, YE[e, j0:j0 + jn, :], ye_.ap[0:jn, slot, :])
                em.barrier()
                chk()
                AR.off = AR_mid
                YA = AR.alloc([128, NE, JL + 1, D], BF16, "YA")
                for e in range(NE):
                    for jc in range(JL):
                        LD(YA, YA.ap[0:JW, e, jc, :], YE[e, jc * JW:(jc + 1) * JW, :])
                    LD(YA, YA.ap[0:capC, e, JL, :], YE[e, capL:NS, :])
                G2 = AR.alloc([128, D], F32, "G2")
                Pg = [AR.alloc([128, JL * JW], BF16, f"Pg{i}") for i in range(2)]
                GT = [AR.alloc([128, JL, 128], BF16, f"GT{i}") for i in range(2)]
                xts = [AR.alloc([128, D], F32, f"xs{i}") for i in range(2)]
                xo = [AR.alloc([128, D], F32, f"xq{i}") for i in range(2)]
                mt = AR.alloc([128, 512], F32, "mt2")
                it = 0
                ip = 0
                for (v, t0, ntl) in streams(b):
                    gate_tile(l, v, 1, G2)
                    is_lat = t0 >= NTC
                    cap = capL if is_lat else capC
                    chunks = [(jc * JW, JW, jc) for jc in range(JL)] if is_lat else [(0, capC, JL)]
                    for i in range(t0, t0 + ntl):
                        xt, xo_ = xts[it % 2], xo[it % 2]
                        it += 1
                        LD(xt, xt.ap, XS[b, i * 128:(i + 1) * 128, :])
                        po0, po1 = PS[0], PS[1]
                        for e in range(NE):
                            pg_, gt_ = Pg[ip % 2], GT[ip % 2]
                            ip += 1
                            V("dve", lambda g, i=i, e=e, cap=cap, pg_=pg_: g.tensor_scalar(out=pg_.ap[:, 0:cap], in0=iota[:, 0:cap], scalar1=RNK.ap[:, i, e:e + 1],
                                                                                        scalar2=GW.ap[:, i, e:e + 1], op0=ALU.is_equal, op1=ALU.mult),
                              r=[cst, RNK, GW], w=[pg_])
                            p = psum()
                            pb = bfv(p)
                            for ci, (j0, jn, slot) in enumerate(chunks):
                                V("pe", lambda g, pb=pb, ci=ci, j0=j0, jn=jn, pg_=pg_: g.transpose(out=pb[0:jn, ci * 128:(ci + 1) * 128], in_=pg_.ap[:, j0:j0 + jn], identity=ident_b),
                                  r=[pg_, cstb], w=[p])
                            jn = chunks[0][1]
                            ncn = len(chunks)
                            V("act", lambda g, pb=pb, jn=jn, ncn=ncn, gt_=gt_: g.copy(out=gt_.ap[0:jn, 0:ncn, :], in_=pb[0:jn, 0:ncn * 128].rearrange("p (c t) -> p c t", c=ncn)),
                              r=[p], w=[gt_])
                            for ci, (j0, jn, slot) in enumerate(chunks):
                                for hf, po in enumerate((po0, po1)):
                                    V("pe", lambda g, po=po, ci=ci, jn=jn, slot=slot, hf=hf, e=e, gt_=gt_, ncn=ncn: g.matmul(
                                        po.ap, lhsT=gt_.ap[0:jn, ci, :], rhs=YA.ap[0:jn, e, slot, hf * 512:(hf + 1) * 512],
                                        start=(e == 0 and ci == 0), stop=(e == NE - 1 and ci == ncn - 1)), r=[gt_, YA], w=[po])
                        for hf, po in enumerate((po0, po1)):
                            tt("dve", mt, mt.ap, po, po.ap, G2, G2.ap[:, hf * 512:(hf + 1) * 512], ALU.mult)
                            tt("dve", xo_, xo_.ap[:, hf * 512:(hf + 1) * 512], mt, mt.ap, xt, xt.ap[:, hf * 512:(hf + 1) * 512], ALU.add)
                        STO(xo_, XS[b, i * 128:(i + 1) * 128, :], xo_.ap)
                em.barrier()
                chk()
                AR.reset()

        fn = AR.alloc([128, D], F32, "fn")
        LD(fn, fn.ap, bc_row(0, I["final_norm"], D))
        xts = [AR.alloc([128, D], F32, f"xf{i}") for i in range(2)]
        junk = AR.alloc([128, D], F32, "junkf")
        stats = [AR.alloc([128, 2], F32, f"sf{i}") for i in range(2)]
        xo = [AR.alloc([128, D], F32, f"xfo{i}") for i in range(2)]
        it = 0
        for b in range(NB):
            for i in range(NTL):
                xt, stt_, xo_ = xts[it % 2], stats[it % 2], xo[it % 2]
                it += 1
                LD(xt, xt.ap, XS[b, C + i * 128:C + (i + 1) * 128, :])
                rms_scale(xt, stt_, junk, D, 1e-6)
                V("dve", lambda g, xt=xt, stt_=stt_, xo_=xo_: g.scalar_tensor_tensor(out=xo_.ap, in0=xt.ap, scalar=stt_.ap[:, 0:1], in1=fn.ap,
                                                                                     op0=ALU.mult, op1=ALU.mult), r=[xt, stt_, fn], w=[xo_])
                STO(xo_, OUT[b, i * 128:(i + 1) * 128, :], xo_.ap)
        em.barrier()
        em.finalize()
    return nc, em


def make_consts(T):
    cst = np.zeros((128, 768), np.float32)
    cst[:, 0:128] = np.eye(128, dtype=np.float32)
    cst[:, 128:256] = np.eye(128, dtype=np.float32)[::-1]
    tp = np.arange(128)[:, None]
    tt_ = np.arange(128)[None, :]
    cst[:, 256:384] = (tp < tt_).astype(np.float32)
    cst[:, 384:512] = 1.0
    cst[:, 512:768] = np.arange(256, dtype=np.float32)[None, :]
    t = np.arange(T)
    row = (t // 64).astype(np.float32)
    col = (t % 64).astype(np.float32)
    inv = (np.float32(10000.0) ** (-np.arange(0, 32, 2, dtype=np.float32) / np.float32(32))).astype(np.float32)
    ang_r = row[:, None] * inv[None, :]
    ang_c = col[:, None] * inv[None, :]
    rope = np.concatenate([np.cos(ang_r), np.cos(ang_c), np.sin(ang_r), np.sin(ang_c)], axis=1).astype(np.float32)
    return cst, rope


_CACHE = {}


def kernel(**inputs):
    x = np.asarray(inputs["x"], np.float32)
    B, T, _ = x.shape
    C = inputs["ctx"].shape[1]
    L = inputs["ada_w"].shape[0]
    NB = 2
    ncores = B // NB
    key = (T, C, L, NB)
    if key not in _CACHE:
        _CACHE[key] = build(T, C, L, NB)[0]
    nc = _CACHE[key]
    cst, rope = make_consts(T)
    shared = {}
    for k, v in inputs.items():
        if k in ("x", "ctx", "c"):
            continue
        a = np.ascontiguousarray(np.asarray(v, np.float32))
        if k in ("c_ctx", "final_norm"):
            a = a.reshape(1, -1)
        shared[k] = a
    shared["cst"] = cst
    shared["rope"] = rope
    in_maps = []
    for i in range(ncores):
        m = dict(shared)
        m["x"] = np.ascontiguousarray(x[i * NB:(i + 1) * NB])
        m["ctx"] = np.ascontiguousarray(np.asarray(inputs["ctx"], np.float32)[i * NB:(i + 1) * NB])
        m["c"] = np.ascontiguousarray(np.asarray(inputs["c"], np.float32)[i * NB:(i + 1) * NB])
        in_maps.append(m)
    res = run_bass_kernel_spmd(nc, in_maps, core_ids=list(range(ncores)))
    return np.concatenate([r["out"] for r in res.results], axis=0).astype(np.float32)
```
